# Optimizing a Trainium2 kernel written in Bass

```python
import jax, jax.numpy as jnp
from jax import lax
import numpy as np

D_MODEL = 1024
BATCH = 4
SEQ = 8192
DEPTH = 2
DEC_BATCH = 16
DEC_SEQ = 32
PAST_LEN = 4096

CHUNK = 64
N_MIX = 2
N_HEADS = 16
HEAD_DIM = D_MODEL // N_HEADS
N_PAST_CHUNKS = 8
BAND_PAST = N_PAST_CHUNKS * CHUNK
BAND = BAND_PAST + CHUNK
REL_CLIP = 128
N_REL = 2 * REL_CLIP + 1
SB_BLOCK = 128
D_FF = D_MODEL * 7 // 2
N_EXPERTS = 8
TOP_K = 2
N_A_LAYERS = (DEPTH + 1) // 2
N_B_LAYERS = DEPTH // 2
EPS = 1e-6
NEG_INF = -1e30

kernel_name = "hybrid_chunkband_stickbreaking_stream_step"


def rms_norm(x, g):
    xf = x.astype(jnp.float32)
    y = xf * lax.rsqrt(jnp.mean(xf * xf, axis=-1, keepdims=True) + EPS)
    return (y * g.astype(jnp.float32)).astype(x.dtype)


def ada_modulation(c, w, b):
    m = jax.nn.silu(c) @ w + b
    return jnp.split(m, 6, axis=-1)


def modulated_norm(x, g, shift, scale):
    h = rms_norm(x, g)
    return h * (1.0 + scale[:, None, :]) + shift[:, None, :]


def split_heads(qkv):
    b, s, _ = qkv.shape
    q, k, v = jnp.split(qkv, 3, axis=-1)
    shp = (b, s, N_HEADS, HEAD_DIM)
    return q.reshape(shp), k.reshape(shp), v.reshape(shp)


def band_attention(q, k, v, q_pos, k_pos, rel_bias):
    s = jnp.einsum('bqhd,bkhd->bhqk', q, k).astype(jnp.float32) * (HEAD_DIM ** -0.5)
    rel = jnp.clip(q_pos[:, None] - k_pos[None, :], -REL_CLIP, REL_CLIP) + REL_CLIP
    s = s + rel_bias.astype(jnp.float32)[:, rel]
    qc = q_pos // CHUNK
    kc = k_pos // CHUNK
    valid = ((k_pos[None, :] >= 0) & (kc[None, :] <= qc[:, None])
             & (kc[None, :] >= qc[:, None] - N_PAST_CHUNKS))
    s = jnp.where(valid, s, NEG_INF)
    p = jax.nn.softmax(s, axis=-1)
    return jnp.einsum('bhqk,bkhd->bqhd', p.astype(v.dtype), v)


def band_attention_prompt(q, k, v, rel_bias):
    b, s = q.shape[0], q.shape[1]
    n_chunks = s // CHUNK
    pad = ((0, 0), (BAND_PAST, 0), (0, 0), (0, 0))
    kp = jnp.pad(k, pad)
    vp = jnp.pad(v, pad)

    def one_chunk(c):
        start = c * CHUNK
        qc = lax.dynamic_slice_in_dim(q, start, CHUNK, axis=1)
        kb = lax.dynamic_slice_in_dim(kp, start, BAND, axis=1)
        vb = lax.dynamic_slice_in_dim(vp, start, BAND, axis=1)
        q_pos = start + jnp.arange(CHUNK, dtype=jnp.int32)
        k_pos = start - BAND_PAST + jnp.arange(BAND, dtype=jnp.int32)
        return band_attention(qc, kb, vb, q_pos, k_pos, rel_bias)

    out = lax.map(one_chunk, jnp.arange(n_chunks, dtype=jnp.int32))
    return jnp.moveaxis(out, 0, 1).reshape(b, s, N_HEADS, HEAD_DIM)


def stick_breaking(q, k, v, q_pos, k_pos):
    z = jnp.einsum('bqhd,bkhd->bhqk', q, k).astype(jnp.float32) * (HEAD_DIM ** -0.5)
    causal = k_pos[None, :] < q_pos[:, None]
    log_keep = jnp.where(causal, jax.nn.log_sigmoid(-z), 0.0)
    after = lax.cumsum(log_keep, axis=3, reverse=True) - log_keep
    a = jnp.where(causal, jnp.exp(jax.nn.log_sigmoid(z) + after), 0.0)
    return jnp.einsum('bhqk,bkhd->bqhd', a.astype(v.dtype), v)


def stick_breaking_blocks(q, k, v, q_pos, k_pos):
    b, nq = q.shape[0], q.shape[1]
    qb = SB_BLOCK if nq % SB_BLOCK == 0 else nq
    nb = nq // qb
    q_blocks = jnp.moveaxis(q.reshape(b, nb, qb, N_HEADS, HEAD_DIM), 1, 0)
    pos_blocks = q_pos.reshape(nb, qb)
    out = lax.map(lambda a: stick_breaking(a[0], k, v, a[1], k_pos), (q_blocks, pos_blocks))
    return jnp.moveaxis(out, 0, 1).reshape(b, nq, N_HEADS, HEAD_DIM)


def swiglu(h, w_gate, w_up, w_down):
    return (jax.nn.silu(h @ w_gate) * (h @ w_up)) @ w_down


def moe_swiglu(h, w_router, w_gate, w_up, w_down):
    logits = (h @ w_router).astype(jnp.float32)
    top_val, top_idx = lax.top_k(logits, TOP_K)
    top_w = jax.nn.softmax(top_val, axis=-1)
    gates = jnp.sum(jax.nn.one_hot(top_idx, N_EXPERTS, dtype=jnp.float32) * top_w[..., None], axis=-2)
    out = jnp.zeros_like(h)
    for e in range(N_EXPERTS):
        out = out + gates[..., e:e + 1].astype(h.dtype) * swiglu(h, w_gate[e], w_up[e], w_down[e])
    return out


def setup_inputs(seed: int = 0) -> dict:
    key = jax.random.key(seed)
    ks = jax.random.split(key, 24)

    def nrm(k, shape, scale):
        return jax.random.normal(k, shape, jnp.float32) * scale

    win = min(BAND_PAST, PAST_LEN)
    d, f = D_MODEL, D_FF
    return {
        "x_prompt": nrm(ks[0], (BATCH, SEQ, d), 1.0),
        "x_sample": nrm(ks[1], (DEC_BATCH, DEC_SEQ, d), 1.0),
        "cache_a_k": nrm(ks[2], (N_A_LAYERS, DEC_BATCH, win, N_HEADS, HEAD_DIM), 1.0),
        "cache_a_v": nrm(ks[3], (N_A_LAYERS, DEC_BATCH, win, N_HEADS, HEAD_DIM), 1.0),
        "cache_b_k": nrm(ks[4], (N_B_LAYERS, DEC_BATCH, PAST_LEN, N_HEADS, HEAD_DIM), 1.0),
        "cache_b_v": nrm(ks[5], (N_B_LAYERS, DEC_BATCH, PAST_LEN, N_HEADS, HEAD_DIM), 1.0),
        "c_prompt": nrm(ks[6], (BATCH, d), 1.0),
        "c_sample": nrm(ks[7], (DEC_BATCH, d), 1.0),
        "w_qkv": nrm(ks[8], (DEPTH, d, 3 * d), d ** -0.5),
        "w_o": nrm(ks[9], (DEPTH, d, d), d ** -0.5),
        "norm1_g": 1.0 + nrm(ks[10], (DEPTH, d), 0.05),
        "norm2_g": 1.0 + nrm(ks[11], (DEPTH, d), 0.05),
        "w_ada": nrm(ks[12], (DEPTH, d, 6 * d), 0.5 * d ** -0.5),
        "b_ada": nrm(ks[13], (DEPTH, 6 * d), 0.02),
        "q_norm_g": 1.0 + nrm(ks[14], (N_A_LAYERS, HEAD_DIM), 0.05),
        "k_norm_g": 1.0 + nrm(ks[15], (N_A_LAYERS, HEAD_DIM), 0.05),
        "rel_bias": nrm(ks[16], (N_A_LAYERS, N_HEADS, N_REL), 0.1),
        "w_gate_d": nrm(ks[17], (N_A_LAYERS, d, f), d ** -0.5),
        "w_up_d": nrm(ks[18], (N_A_LAYERS, d, f), d ** -0.5),
        "w_down_d": nrm(ks[19], (N_A_LAYERS, f, d), f ** -0.5),
        "w_router": nrm(ks[20], (N_B_LAYERS, d, N_EXPERTS), d ** -0.5),
        "w_gate_e": nrm(ks[21], (N_B_LAYERS, N_EXPERTS, d, f), d ** -0.5),
        "w_up_e": nrm(ks[22], (N_B_LAYERS, N_EXPERTS, d, f), d ** -0.5),
        "w_down_e": nrm(ks[23], (N_B_LAYERS, N_EXPERTS, f, d), f ** -0.5),
    }


def reference(x_prompt, x_sample, cache_a_k, cache_a_v, cache_b_k, cache_b_v, c_prompt, c_sample,
              w_qkv, w_o, norm1_g, norm2_g, w_ada, b_ada, q_norm_g, k_norm_g, rel_bias,
              w_gate_d, w_up_d, w_down_d, w_router, w_gate_e, w_up_e, w_down_e):
    bp, sp = x_prompt.shape[0], x_prompt.shape[1]
    bs, ts = x_sample.shape[0], x_sample.shape[1]
    past_len = cache_b_k.shape[2]
    win = cache_a_k.shape[2]
    keep_p = min(BAND_PAST, sp)
    pos_p = jnp.arange(sp, dtype=jnp.int32)
    pos_s = past_len + jnp.arange(ts, dtype=jnp.int32)
    pos_win = past_len - win + jnp.arange(win, dtype=jnp.int32)
    pos_all_b = jnp.arange(past_len + ts, dtype=jnp.int32)

    a_k_p, a_v_p, a_k_s, a_v_s = [], [], [], []
    b_k_p, b_v_p, b_k_s, b_v_s = [], [], [], []
    xp, xs = x_prompt, x_sample
    for i in range(DEPTH):
        j = i // N_MIX
        sh1p, sc1p, g1p, sh2p, sc2p, g2p = ada_modulation(c_prompt, w_ada[i], b_ada[i])
        sh1s, sc1s, g1s, sh2s, sc2s, g2s = ada_modulation(c_sample, w_ada[i], b_ada[i])

        qp, kp, vp = split_heads(modulated_norm(xp, norm1_g[i], sh1p, sc1p) @ w_qkv[i])
        qs, ks, vs = split_heads(modulated_norm(xs, norm1_g[i], sh1s, sc1s) @ w_qkv[i])
        if i % N_MIX == 0:
            qp, kp = rms_norm(qp, q_norm_g[j]), rms_norm(kp, k_norm_g[j])
            qs, ks = rms_norm(qs, q_norm_g[j]), rms_norm(ks, k_norm_g[j])
            op = band_attention_prompt(qp, kp, vp, rel_bias[j])
            k_all = jnp.concatenate([cache_a_k[j], ks], axis=1)
            v_all = jnp.concatenate([cache_a_v[j], vs], axis=1)
            os_ = band_attention(qs, k_all, v_all, pos_s, jnp.concatenate([pos_win, pos_s]), rel_bias[j])
            a_k_p.append(kp[:, sp - keep_p:])
            a_v_p.append(vp[:, sp - keep_p:])
            a_k_s.append(k_all[:, ts:])
            a_v_s.append(v_all[:, ts:])
        else:
            op = stick_breaking_blocks(qp, kp, vp, pos_p, pos_p)
            k_all = jnp.concatenate([cache_b_k[j], ks], axis=1)
            v_all = jnp.concatenate([cache_b_v[j], vs], axis=1)
            os_ = stick_breaking_blocks(qs, k_all, v_all, pos_s, pos_all_b)
            b_k_p.append(kp)
            b_v_p.append(vp)
            b_k_s.append(ks)
            b_v_s.append(vs)
        xp = xp + g1p[:, None, :] * (op.reshape(bp, sp, D_MODEL) @ w_o[i])
        xs = xs + g1s[:, None, :] * (os_.reshape(bs, ts, D_MODEL) @ w_o[i])

        hp = modulated_norm(xp, norm2_g[i], sh2p, sc2p)
        hs = modulated_norm(xs, norm2_g[i], sh2s, sc2s)
        if i % 2 == 0:
            fp = swiglu(hp, w_gate_d[j], w_up_d[j], w_down_d[j])
            fs = swiglu(hs, w_gate_d[j], w_up_d[j], w_down_d[j])
        else:
            fp = moe_swiglu(hp, w_router[j], w_gate_e[j], w_up_e[j], w_down_e[j])
            fs = moe_swiglu(hs, w_router[j], w_gate_e[j], w_up_e[j], w_down_e[j])
        xp = xp + g2p[:, None, :] * fp
        xs = xs + g2s[:, None, :] * fs

    return (xp, xs,
            jnp.stack(a_k_p), jnp.stack(a_v_p), jnp.stack(a_k_s), jnp.stack(a_v_s),
            jnp.stack(b_k_p), jnp.stack(b_v_p), jnp.stack(b_k_s), jnp.stack(b_v_s))
```

```python
import numpy as np
from contextlib import ExitStack
import concourse.bass as bass
import concourse.mybir as mybir
from concourse.bass_utils import run_bass_kernel_spmd

F32 = mybir.dt.float32
BF16 = mybir.dt.bfloat16
AF = mybir.ActivationFunctionType
ALU = mybir.AluOpType
AX = mybir.AxisListType

D = 1024
NT = 8192
TS = 512
NTILE = NT // TS
SMP = 64
NTOT = NT + SMP
FF = 3584
NFC = FF // 512
NE = 8
WRELW = 768
EPS = 1e-6


class Res:
    __slots__ = ("w", "r")

    def __init__(self):
        self.w = None
        self.r = {}


class VEng:
    def __init__(self, name, hw, sems, inc, inorder):
        self.name = name
        self.hw = hw
        self.sems = sems
        self.inc = inc
        self.n = 0
        self.inorder = inorder


class HwQ:
    def __init__(self, name, h):
        self.name = name
        self.h = h
        self.waited = {}
        self.waited_any = {}


class Sched:
    def __init__(self, nc, es, ns_compute=8, ns_dma=24):
        self.nc = nc
        self.nwaits = 0
        self.ninst = 0

        def sems(prefix, n):
            return [es.enter_context(nc.semaphore(f"{prefix}{i}")) for i in range(n)]

        self.q_pe = HwQ("pe", nc.tensor)
        self.q_act = HwQ("act", nc.scalar)
        self.q_dve = HwQ("dve", nc.vector)
        self.q_pool = HwQ("pool", nc.gpsimd)
        self.q_sp = HwQ("sp", nc.sync)
        self.PE = VEng("PE", self.q_pe, sems("spe", ns_compute), 1, True)
        self.ACT = VEng("ACT", self.q_act, sems("sact", ns_compute), 1, True)
        self.DVE = VEng("DVE", self.q_dve, sems("sdve", ns_compute), 1, True)
        self.POOL = VEng("POOL", self.q_pool, sems("spool", ns_compute), 1, True)
        self.DSP = VEng("DSP", self.q_sp, sems("sdsp", ns_dma), 16, False)
        self.DPOOL = VEng("DPOOL", self.q_pool, sems("sdpool", ns_dma), 16, False)

    def _wait(self, hw, ve, seq):
        ns = len(ve.sems)
        slot = seq % ns
        if ve.inorder and hw.waited_any.get(ve.name, -1) >= seq:
            return
        if hw.waited.get((ve.name, slot), -1) >= seq:
            return
        hw.h.wait_ge(ve.sems[slot], (seq // ns + 1) * ve.inc)
        self.nwaits += 1
        hw.waited[(ve.name, slot)] = seq
        if ve.inorder:
            hw.waited_any[ve.name] = max(hw.waited_any.get(ve.name, -1), seq)

    def issue(self, ve, fn, reads=(), writes=()):
        dl = []
        for r in reads:
            if r.w is not None:
                dl.append(r.w)
        for w in writes:
            if w.w is not None:
                dl.append(w.w)
            dl.extend(w.r.values())
        best = {}
        for e, s in dl:
            if e.inorder:
                if best.get(e, (None, -1))[1] < s:
                    best[e] = (e, s)
            else:
                best[(e, s)] = (e, s)
        hw = ve.hw
        if not ve.inorder and ve.n >= len(ve.sems):
            self._wait(hw, ve, ve.n - len(ve.sems))
        for e, s in best.values():
            if e is ve and ve.name == "PE":
                continue
            self._wait(hw, e, s)
        ins = fn(hw.h)
        seq = ve.n
        ve.n += 1
        ns = len(ve.sems)
        ins.then_inc(ve.sems[seq % ns], ve.inc)
        self.ninst += 1
        for r in reads:
            r.r[ve if ve.inorder else (ve, seq)] = (ve, seq)
        for w in writes:
            w.w = (ve, seq)
            w.r = {}
        return seq

    def barrier(self):
        vengs = [self.PE, self.ACT, self.DVE, self.POOL, self.DSP, self.DPOOL]
        for hw in (self.q_pe, self.q_act, self.q_dve, self.q_pool, self.q_sp):
            for ve in vengs:
                if ve.n == 0:
                    continue
                if ve.inorder:
                    if ve.hw is hw and ve.name == "PE":
                        continue
                    self._wait(hw, ve, ve.n - 1)
                else:
                    for q in range(max(0, ve.n - len(ve.sems)), ve.n):
                        self._wait(hw, ve, q)

    def final_wait(self, hwq, vengs):
        for ve in vengs:
            ns = len(ve.sems)
            if ve.n == 0:
                continue
            last = ve.n - 1
            for slot in range(ns):
                s = last - ((last - slot) % ns)
                if s >= 0:
                    hwq.h.wait_ge(ve.sems[slot], (s // ns + 1) * ve.inc)


class Rot:
    def __init__(self, items):
        self.items = items
        self.i = 0

    def next(self):
        it = self.items[self.i % len(self.items)]
        self.i += 1
        return it


class RotView:
    def __init__(self, rot, fn):
        self.rot = rot
        self.fn = fn

    def next(self):
        t, r = self.rot.next()
        return self.fn(t), r


def build_program(n_layers=2, stop_after=None, debug=False):
    nc = bass.Bass("TRN2", target_bir_lowering=False)

    def din(name, shape):
        return nc.dram_tensor(name, list(shape), F32, kind="ExternalInput").ap()

    def dout(name, shape):
        return nc.dram_tensor(name, list(shape), F32, kind="ExternalOutput").ap()

    def dscr(name, shape, dt):
        return nc.dram_tensor(name, list(shape), dt, kind=("ExternalOutput" if debug else "Internal")).ap()

    xp = din("xp", [NT, D])
    xs = din("xs", [SMP, D])
    cak = din("cak", [2, 512, D])
    cav = din("cav", [2, 512, D])
    cbk = din("cbk", [2, 4096, D])
    cbv = din("cbv", [2, 4096, D])
    cpl = din("cpl", [128, 8])
    csl = din("csl", [128, 8, 2])
    w_qkv = din("w_qkv", [2, D, 3 * D])
    w_o = din("w_o", [2, D, D])
    n1g = din("n1g", [2, D])
    n2g = din("n2g", [2, D])
    w_ada = din("w_ada", [2, D, 6 * D])
    b_ada = din("b_ada", [2, 6 * D])
    qng = din("qng", [1, 64])
    kng = din("kng", [1, 64])
    wrel = din("wrel", [16, 128, WRELW])
    wgd = din("wgd", [1, D, FF])
    wud = din("wud", [1, D, FF])
    wdd = din("wdd", [1, FF, D])
    wrt = din("wrt", [D, NE])
    wge = din("wge", [NE, D, FF])
    wue = din("wue", [NE, D, FF])
    wde = din("wde", [NE, FF, D])
    ident_d = din("ident", [128, 128])
    negtri_d = din("negtri", [128, 128])
    msb_d = din("msb", [128, 8, 512])
    msmp_d = din("msmp", [32, 32])
    pm_d = din("pm", [128, 2])

    yp = dout("yp", [NT // 2, D])
    ys = dout("ys", [SMP, D])
    akp = dout("akp", [512, D])
    avp = dout("avp", [512, D])
    aks = dout("aks", [2, 512, D])
    avs = dout("avs", [2, 512, D])
    bkp = dout("bkp", [NT, D])
    bvp = dout("bvp", [NT, D])
    bks = dout("bks", [SMP, D])
    bvs = dout("bvs", [SMP, D])

    QT = [dscr(f"QT{l}", [8, 128, NTOT], BF16) for l in range(2)]
    KT = [dscr(f"KT{l}", [8, 128, NTOT], BF16) for l in range(2)]
    VB = [dscr("VB0", [NTOT, D], BF16), None]
    VBm = dscr("VBm", [8, 128, NT // 128 + 1, 128], BF16)
    X1 = dscr("X1", [NTOT, D], F32)
    X2 = dscr("X2", [NTOT, D], F32)
    X3 = dscr("X3", [NT // 2 + SMP, D], F32)
    XN2T = [dscr("XN2T0", [8, 128, NTOT], BF16), dscr("XN2T1", [8, 128, NT // 2 + SMP], BF16)]
    CKT = dscr("CKT", [2, 8, 128, 4096], BF16)

    def rl(n):
        return [Res() for _ in range(n)]

    r_QT = [rl(17), rl(17)]
    r_KT = [rl(17), rl(17)]
    r_VB = [rl(17), rl(17)]
    r_X1 = rl(17)
    r_X2 = rl(17)
    r_X3 = rl(9)
    r_XN2T = [rl(17), rl(9)]
    r_CKT = rl(2)
    r_out = Res()

    with ExitStack() as top:
        S = Sched(nc, top)
        I = S.issue
        PE, ACT, DVE, POOL, DSP, DPOOL = S.PE, S.ACT, S.DVE, S.POOL, S.DSP, S.DPOOL

        uniq = [0]

        def sbt(es, name, shape, dt):
            uniq[0] += 1
            return es.enter_context(nc.sbuf_tensor(f"sb{uniq[0]}_{name}", list(shape), dt)), Res()

        def pst(es, name, shape, dt):
            uniq[0] += 1
            return es.enter_context(nc.psum_tensor(f"ps{uniq[0]}_{name}", list(shape), dt)), Res()

        identf, r_identf = sbt(top, "identf", [128, 128], F32)
        identb, r_identb = sbt(top, "identb", [128, 128], BF16)
        onesb, r_onesb = sbt(top, "onesb", [128, 128], BF16)
        negones, r_negones = sbt(top, "negones", [128, 128], BF16)
        negtri, r_negtri = sbt(top, "negtri", [128, 128], BF16)
        pm, r_pm = sbt(top, "pm", [128, 2], F32)
        I(DSP, lambda h: h.dma_start(out=identf[:], in_=ident_d), writes=[r_identf])
        I(DSP, lambda h: h.dma_start(out=pm[:], in_=pm_d), writes=[r_pm])
        I(DPOOL, lambda h: h.dma_start(out=identb[:], in_=ident_d), writes=[r_identb])
        I(DPOOL, lambda h: h.dma_start(out=negtri[:], in_=negtri_d), writes=[r_negtri])
        I(POOL, lambda h: h.memset(onesb[:], 1.0), writes=[r_onesb])
        I(POOL, lambda h: h.memset(negones[:], -1.0), writes=[r_negones])

        evac_flip = [0]
        dumped = set()

        def dump(name, ap, res, shape, dt=F32):
            if not debug or name in dumped:
                return
            dumped.add(name)
            d = nc.dram_tensor("dbg_" + name, list(shape), dt, kind="ExternalOutput").ap()
            I(DSP, lambda h: h.dma_start(out=d, in_=ap), reads=[res], writes=[Res()])

        def evac(out_ap, in_ap, reads, writes, eng=None):
            if eng is None:
                eng = ACT if (evac_flip[0] % 2 == 0) else DVE
                evac_flip[0] += 1
            if eng is ACT:
                I(ACT, lambda h: h.copy(out=out_ap, in_=in_ap), reads=reads, writes=writes)
            else:
                I(eng, lambda h: h.tensor_copy(out=out_ap, in_=in_ap), reads=reads, writes=writes)

        def tok_rows(t):
            if t < NTILE:
                return t * TS, TS, 4, 128
            return NT, SMP, 1, SMP

        for l in range(n_layers):
            with ExitStack() as lay:
                G2 = [sbt(lay, f"G2_{s}", [128, D], F32) for s in range(2)]
                gates, r_gates = sbt(lay, "gates", [128, 33, NE], F32)
                with ExitStack() as att:
                    AD = [[None] * 6 for _ in range(2)]
                    a1s = att.enter_context(ExitStack())
                    for s in range(2):
                        for v in (2, 3, 4):
                            AD[s][v] = sbt(att, f"AD{s}_{v}", [128, D], F32)
                        AD[s][5] = G2[s]
                    for s in range(2):
                        for v in (0, 1):
                            AD[s][v] = sbt(a1s, f"AD{s}_{v}", [128, D], F32)

                    with ExitStack() as es:
                        cp_t, r_cp = sbt(es, "cp_t", [128, 8], F32)
                        cs_t, r_cs = sbt(es, "cs_t", [128, 8, 2], F32)
                        LP, r_LP = sbt(es, "LP", [128, 8, 128], BF16)
                        LS, r_LS = sbt(es, "LS", [128, 8, 64], BF16)
                        bb, r_bb = sbt(es, "bb", [128, 6 * D], F32)
                        ng1, r_ng1 = sbt(es, "ng1", [128, D], F32)
                        ng2, r_ng2 = sbt(es, "ng2", [128, D], F32)
                        wch = Rot([sbt(es, f"wch{i}", [128, 8, 512], BF16) for i in range(2)])
                        pA = Rot([pst(es, f"pA{i}", [128, 512], F32) for i in range(4)])
                        I(DSP, lambda h: h.dma_start(out=cp_t[:], in_=cpl), writes=[r_cp])
                        I(DSP, lambda h: h.dma_start(out=cs_t[:], in_=csl), writes=[r_cs])
                        I(DSP, lambda h: h.dma_start(out=bb[:], in_=b_ada[l, :].partition_broadcast(128)), writes=[r_bb])
                        I(DSP, lambda h: h.dma_start(out=ng1[:], in_=n1g[l, :].partition_broadcast(128)), writes=[r_ng1])
                        I(DSP, lambda h: h.dma_start(out=ng2[:], in_=n2g[l, :].partition_broadcast(128)), writes=[r_ng2])
                        I(ACT, lambda h: h.activation(out=cp_t[:], in_=cp_t[:], func=AF.Silu), reads=[r_cp], writes=[r_cp])
                        I(ACT, lambda h: h.activation(out=cs_t[:], in_=cs_t[:], func=AF.Silu), reads=[r_cs], writes=[r_cs])
                        for k in range(8):
                            I(DVE, lambda h: h.tensor_scalar(out=LP[:, k, :], in0=onesb[:, :], scalar1=cp_t[:, k:k + 1],
                                                             scalar2=None, op0=ALU.mult),
                              reads=[r_onesb, r_cp], writes=[r_LP])
                            for s in range(2):
                                I(DVE, lambda h: h.tensor_scalar(out=LS[:, k, s * 32:(s + 1) * 32], in0=onesb[:, 0:32],
                                                                 scalar1=cs_t[:, k, s:s + 1], scalar2=None, op0=ALU.mult),
                                  reads=[r_onesb, r_cs], writes=[r_LS])
                        for c in range(12):
                            wt, r_wt = wch.next()
                            I(DPOOL, lambda h: h.dma_start(
                                out=wt[:], in_=w_ada[l, :, c * 512:(c + 1) * 512].rearrange("(k p) n -> p k n", p=128)),
                              writes=[r_wt])
                            vec, half = c // 2, c % 2
                            cols = slice(half * 512, (half + 1) * 512)
                            for s, (L_, r_L, P_) in enumerate(((LP, r_LP, 128), (LS, r_LS, 64))):
                                ps, r_ps = pA.next()
                                for k in range(8):
                                    I(PE, lambda h: h.matmul(ps[:P_, :], lhsT=L_[:, k, :P_], rhs=wt[:, k, :],
                                                             start=(k == 0), stop=(k == 7)),
                                      reads=[r_L, r_wt], writes=[r_ps])
                                dst, r_dst = AD[s][vec]
                                I(DVE, lambda h: h.tensor_tensor(out=dst[:P_, cols], in0=ps[:P_, :],
                                                                 in1=bb[:P_, c * 512:(c + 1) * 512], op=ALU.add),
                                  reads=[r_ps, r_bb], writes=[r_dst])
                                if vec in (1, 4):
                                    ng, r_ng = (ng1, r_ng1) if vec == 1 else (ng2, r_ng2)
                                    I(DVE, lambda h: h.scalar_tensor_tensor(out=dst[:P_, cols], in0=dst[:P_, cols], scalar=1.0,
                                                                            in1=ng[:P_, cols], op0=ALU.add, op1=ALU.mult),
                                      reads=[r_dst, r_ng], writes=[r_dst])

                    S.barrier()
                    for v in range(6):
                        dump(f"AD0_{v}_l{l}", AD[0][v][0][:, :], AD[0][v][1], [128, D])
                        dump(f"AD1_{v}_l{l}", AD[1][v][0][:64, :], AD[1][v][1], [64, D])
                    def norm_to_T(es_bufs, x_ap, r_x, GM, SH, P_, defer_T=None):
                        (junk, r_junk), ssr, tmpr, xnr, pTr, xnTr = es_bufs
                        ss, r_ss = ssr.next()
                        tmpf, r_tmpf = tmpr.next()
                        xn, r_xn = xnr.next()
                        pT, r_pT = pTr.next()
                        xnT, r_xnT = xnTr.next()
                        I(ACT, lambda h: h.activation(out=junk[:P_, :], in_=x_ap, func=AF.Square, accum_out=ss[:P_, 0:1]),
                          reads=[r_x], writes=[r_junk, r_ss])
                        I(ACT, lambda h: h.activation(out=ss[:P_, 1:2], in_=ss[:P_, 0:1], func=AF.Ln, scale=1.0 / D, bias=EPS),
                          reads=[r_ss], writes=[r_ss])
                        I(ACT, lambda h: h.activation(out=ss[:P_, 2:3], in_=ss[:P_, 1:2], func=AF.Exp, scale=-0.5),
                          reads=[r_ss], writes=[r_ss])
                        I(DVE, lambda h: h.scalar_tensor_tensor(out=tmpf[:P_, :], in0=x_ap, scalar=ss[:P_, 2:3],
                                                                in1=GM[0][:P_, :], op0=ALU.mult, op1=ALU.mult),
                          reads=[r_x, r_ss, GM[1]], writes=[r_tmpf])
                        I(POOL, lambda h: h.tensor_tensor(out=xn[:P_, :], in0=tmpf[:P_, :], in1=SH[0][:P_, :], op=ALU.add),
                          reads=[r_tmpf, SH[1]], writes=[r_xn])
                        def tpart():
                            for k in range(8):
                                I(PE, lambda h: h.transpose(out=pT[:, k, :P_], in_=xn[:P_, k * 128:(k + 1) * 128],
                                                            identity=identb[:P_, :P_]),
                                  reads=[r_xn, r_identb], writes=[r_pT])
                            evac(xnT[:, :, :P_], pT[:, :, :P_], [r_pT], [r_xnT])
                        if defer_T is not None:
                            defer_T.append(tpart)
                            return xnT, r_xnT
                        tpart()
                        dump(f"ss_l{l}", ss[:, :], r_ss, [128, 4])
                        dump(f"tmpf_l{l}", tmpf[:, :], r_tmpf, [128, D])
                        dump(f"xn_l{l}", xn[:, :], r_xn, [128, D], BF16)
                        dump(f"xnT_l{l}", xnT[:, :, :], r_xnT, [128, 8, 128], BF16)
                        return xnT, r_xnT

                    def norm_bufs(es, pfx, npT=1):
                        return (sbt(es, pfx + "junk", [128, D], F32),
                                Rot([sbt(es, f"{pfx}ss{i}", [128, 4], F32) for i in range(2)]),
                                Rot([sbt(es, f"{pfx}tmpf{i}", [128, D], F32) for i in range(1)]),
                                Rot([sbt(es, f"{pfx}xn{i}", [128, D], BF16) for i in range(2)]),
                                Rot([pst(es, f"{pfx}pT{i}", [128, 8, 128], BF16) for i in range(npT)]),
                                Rot([sbt(es, f"{pfx}xnT{i}", [128, 8, 128], BF16) for i in range(2)]))

                    x_src, r_xsrc = ((xp, xs), None) if l == 0 else (None, r_X2)

                    def x_rows(row0, P_, t):
                        if l == 0:
                            if t < NTILE:
                                return xp[row0:row0 + P_, :], []
                            return xs[row0 - NT:row0 - NT + P_, :], []
                        return X2[row0:row0 + P_, :], [r_X2[t]]

                    with ExitStack() as es:
                        wq, r_wq = sbt(es, "wq", [128, 8, 3 * D], BF16)
                        for c in range(6):
                            I(DPOOL, lambda h: h.dma_start(
                                out=wq[:, :, c * 512:(c + 1) * 512],
                                in_=w_qkv[l, :, c * 512:(c + 1) * 512].rearrange("(k p) n -> p k n", p=128)),
                              writes=[r_wq])
                        nb = norm_bufs(es, "a1", 2)
                        xbuf = Rot([sbt(es, f"a1x{i}", [128, D], F32) for i in range(3)])
                        qkvf = Rot([sbt(es, f"qkvf{i}", [128, 3 * D], F32) for i in range(2)])
                        qkb = Rot([sbt(es, f"qkb{i}", [128, 2 * D], BF16) for i in range(2)])
                        vbb = Rot([sbt(es, f"vbb{i}", [128, D], BF16) for i in range(2)])
                        qkT = Rot([sbt(es, f"qkT{i}", [128, 16, 128], BF16) for i in range(2)])
                        pQ = Rot([pst(es, f"pQ{i}", [128, 512], F32) for i in range(3)])
                        pQT = Rot([pst(es, f"pQT{i}", [128, 8, 128], BF16) for i in range(2)])
                        if l == 0:
                            gq, r_gq = sbt(es, "gq", [128, 64], F32)
                            gk, r_gk = sbt(es, "gk", [128, 64], F32)
                            sqb, r_sqb = sbt(es, "sqb", [128, 2 * D], F32)
                            ssq, r_ssq = sbt(es, "ssq", [128, 32], F32)
                            I(DSP, lambda h: h.dma_start(out=gq[:], in_=qng[0, :].partition_broadcast(128)), writes=[r_gq])
                            I(DSP, lambda h: h.dma_start(out=gk[:], in_=kng[0, :].partition_broadcast(128)), writes=[r_gk])
                            I(DVE, lambda h: h.tensor_scalar(out=gq[:], in0=gq[:], scalar1=0.125, scalar2=None, op0=ALU.mult),
                              reads=[r_gq], writes=[r_gq])
                        subs_ = []
                        for t in range(17):
                            row0, ntok, nsub, P_ = tok_rows(t)
                            for sub in range(nsub):
                                subs_.append((t, sub, row0, P_))
                        stA1 = [dict() for _ in subs_]

                        deferred = []
                        TL_ = []

                        def DI(*a, **k):
                            deferred.append((a, k))

                        def flush_():
                            for a, k in deferred:
                                I(*a, **k)
                            deferred.clear()

                        def L_(n_):
                            t, sub, row0, P_ = subs_[n_]
                            r0 = row0 + sub * 128
                            xb, r_xb = xbuf.next()
                            src, rsrc = x_rows(r0, P_, t)
                            I(DSP, lambda h: h.dma_start(out=xb[:P_, :], in_=src), reads=rsrc, writes=[r_xb])
                            stA1[n_]['xb'] = (xb, r_xb)

                        def F_(n_):
                            t, sub, row0, P_ = subs_[n_]
                            r0 = row0 + sub * 128
                            stt_ = stA1[n_]
                            xb, r_xb = stt_['xb']
                            xnT, r_xnT = norm_to_T(nb, xb[:P_, :], r_xb, AD[1 if t == 16 else 0][1], AD[1 if t == 16 else 0][0], P_,
                                                   defer_T=TL_)
                            stt_['xnT'] = (xnT, r_xnT)

                        def G1_(n_):
                            t, sub, row0, P_ = subs_[n_]
                            r0 = row0 + sub * 128
                            stt_ = stA1[n_]
                            xnT, r_xnT = stt_['xnT']
                            qf, r_qf = qkvf.next()
                            for cc in range(6):
                                ps, r_ps = pQ.next()
                                for k in range(8):
                                    I(PE, lambda h: h.matmul(ps[:P_, :], lhsT=xnT[:, k, :P_], rhs=wq[:, k, cc * 512:(cc + 1) * 512],
                                                             start=(k == 0), stop=(k == 7)),
                                      reads=[r_xnT, r_wq], writes=[r_ps])
                                evac(qf[:P_, cc * 512:(cc + 1) * 512], ps[:P_, :], [r_ps], [r_qf])
                            stt_['qf'] = (qf, r_qf)

                        def G2a_(n_):
                            t, sub, row0, P_ = subs_[n_]
                            r0 = row0 + sub * 128
                            stt_ = stA1[n_]
                            qf, r_qf = stt_['qf']
                            qb, r_qb = qkb.next()
                            vb, r_vb = vbb.next()
                            if l == 0:
                                I(ACT, lambda h: h.activation(out=sqb[:P_, :], in_=qf[:P_, 0:2 * D], func=AF.Square),
                                  reads=[r_qf], writes=[r_sqb])
                                I(DVE, lambda h: h.tensor_reduce(out=ssq[:P_, :], in_=sqb[:P_, :].rearrange("p (h d) -> p h d", d=64),
                                                                 axis=AX.X, op=ALU.add),
                                  reads=[r_sqb], writes=[r_ssq])
                                I(ACT, lambda h: h.activation(out=ssq[:P_, :], in_=ssq[:P_, :], func=AF.Ln, scale=1.0 / 64, bias=EPS),
                                  reads=[r_ssq], writes=[r_ssq])
                                I(ACT, lambda h: h.activation(out=ssq[:P_, :], in_=ssq[:P_, :], func=AF.Exp, scale=-0.5),
                                  reads=[r_ssq], writes=[r_ssq])
                                for qi, (g_, r_g) in enumerate(((gq, r_gq), (gk, r_gk))):
                                    fv = qf[:P_, qi * D:(qi + 1) * D].rearrange("p (h d) -> p h d", d=64)
                                    I(DVE, lambda h: h.tensor_tensor(
                                        out=fv, in0=fv,
                                        in1=ssq[:P_, qi * 16:(qi + 1) * 16].unsqueeze(2).to_broadcast([P_, 16, 64]), op=ALU.mult),
                                      reads=[r_qf, r_ssq], writes=[r_qf])
                                    I(DVE, lambda h: h.tensor_tensor(
                                        out=fv, in0=fv, in1=g_[:P_, :].unsqueeze(1).to_broadcast([P_, 16, 64]), op=ALU.mult),
                                      reads=[r_qf, r_g], writes=[r_qf])
                                I(ACT, lambda h: h.copy(out=qb[:P_, :], in_=qf[:P_, 0:2 * D]), reads=[r_qf], writes=[r_qb])
                            else:
                                I(ACT, lambda h: h.activation(out=qb[:P_, 0:D], in_=qf[:P_, 0:D], func=AF.Copy, scale=0.125),
                                  reads=[r_qf], writes=[r_qb])
                                I(POOL, lambda h: h.tensor_copy(out=qb[:P_, D:2 * D], in_=qf[:P_, D:2 * D]), reads=[r_qf], writes=[r_qb])
                            I(POOL if l == 0 else DVE, lambda h: h.tensor_copy(out=vb[:P_, :], in_=qf[:P_, 2 * D:3 * D]), reads=[r_qf], writes=[r_vb])
                            if l == 0:
                                if t == NTILE - 1:
                                    DI(DSP, lambda h: h.dma_start(out=akp[sub * 128:(sub + 1) * 128, :], in_=qf[:P_, D:2 * D]),
                                      reads=[r_qf], writes=[r_out])
                                    DI(DSP, lambda h: h.dma_start(out=avp[sub * 128:(sub + 1) * 128, :], in_=qf[:P_, 2 * D:3 * D]),
                                      reads=[r_qf], writes=[r_out])
                                if t == 16:
                                    for s in range(2):
                                        DI(DSP, lambda h, s=s: h.dma_start(out=aks[s, 480:512, :], in_=qf[s * 32:(s + 1) * 32, D:2 * D]),
                                          reads=[r_qf], writes=[r_out])
                                        DI(DSP, lambda h, s=s: h.dma_start(out=avs[s, 480:512, :], in_=qf[s * 32:(s + 1) * 32, 2 * D:3 * D]),
                                          reads=[r_qf], writes=[r_out])
                                        DI(DSP, lambda h, s=s: h.dma_start(out=aks[s, 0:480, :], in_=cak[s, 32:512, :]), writes=[r_out])
                                        DI(DSP, lambda h, s=s: h.dma_start(out=avs[s, 0:480, :], in_=cav[s, 32:512, :]), writes=[r_out])
                            else:
                                if t < NTILE:
                                    DI(DSP, lambda h: h.dma_start(out=bkp[r0:r0 + 128, :], in_=qf[:P_, D:2 * D]), reads=[r_qf], writes=[r_out])
                                    DI(DSP, lambda h: h.dma_start(out=bvp[r0:r0 + 128, :], in_=qf[:P_, 2 * D:3 * D]), reads=[r_qf], writes=[r_out])
                                else:
                                    DI(DSP, lambda h: h.dma_start(out=bks[:, :], in_=qf[:P_, D:2 * D]), reads=[r_qf], writes=[r_out])
                                    DI(DSP, lambda h: h.dma_start(out=bvs[:, :], in_=qf[:P_, 2 * D:3 * D]), reads=[r_qf], writes=[r_out])
                            if l == 0:
                                DI(DSP, lambda h: h.dma_start(out=VB[0][r0:r0 + P_, :], in_=vb[:P_, :]), reads=[r_vb], writes=[r_VB[0][t]])
                            else:
                                DI(DSP, lambda h: h.dma_start(out=VBm[:, 0:P_, r0 // 128, :].rearrange("m p f -> p m f"),
                                                             in_=vb[:P_, :].rearrange("p (m f) -> p m f", f=128)),
                                  reads=[r_vb], writes=[r_VB[1][t]])
                            stt_['qb'] = (qb, r_qb)

                        def G2b_(n_):
                            t, sub, row0, P_ = subs_[n_]
                            r0 = row0 + sub * 128
                            stt_ = stA1[n_]
                            qb, r_qb = stt_['qb']
                            qt, r_qt = qkT.next()
                            for hf in range(2):
                                pt, r_pt = pQT.next()
                                for m in range(8):
                                    I(PE, lambda h: h.transpose(out=pt[:, m, :P_], in_=qb[:P_, hf * D + m * 128: hf * D + (m + 1) * 128],
                                                                identity=identb[:P_, :P_]),
                                      reads=[r_qb, r_identb], writes=[r_pt])
                                evac(qt[:, hf * 8:(hf + 1) * 8, :P_], pt[:, :, :P_], [r_pt], [r_qt])
                            DI(DSP, lambda h: h.dma_start(out=QT[l][:, :, r0:r0 + P_].rearrange("m p t -> p m t"), in_=qt[:, 0:8, :P_]),
                              reads=[r_qt], writes=[r_QT[l][t]])
                            DI(DSP, lambda h: h.dma_start(out=KT[l][:, :, r0:r0 + P_].rearrange("m p t -> p m t"), in_=qt[:, 8:16, :P_]),
                              reads=[r_qt], writes=[r_KT[l][t]])

                        N_ = len(subs_)
                        for step_ in range(N_ + 4):
                            flush_()
                            if 0 <= step_ - 3 < N_:
                                G2a_(step_ - 3)
                            if 0 <= step_ - 2 < N_:
                                G1_(step_ - 2)
                            if 0 <= step_ - 1 < N_:
                                F_(step_ - 1)
                            if step_ < N_:
                                L_(step_)
                            if 0 <= step_ - 3 < N_:
                                G2b_(step_ - 3)
                            for f_ in TL_:
                                f_()
                            TL_.clear()
                        flush_()
                    S.barrier()
                    a1s.close()
                    if stop_after == ("A1", l):
                        break

                    with ExitStack() as es:
                        wo, r_wo = sbt(es, "wo", [128, 8, D], BF16)
                        I(DPOOL, lambda h: h.dma_start(out=wo[:], in_=w_o[l].rearrange("(k p) n -> p k n", p=128)), writes=[r_wo])
                        nb = norm_bufs(es, "a2", 1)
                        xbuf = Rot([sbt(es, f"a2x{i}", [128, D], F32) for i in range(4)])
                        x1b = Rot([sbt(es, f"a2x1{i}", [128, D], F32) for i in range(2)])
                        OTs, r_OTs = sbt(es, "OTs", [128, 8, TS], BF16)
                        pY = Rot([pst(es, f"pY{i}", [128, 512], F32) for i in range(3 if l == 0 else 4)])
                        if l == 1:
                            wrf, r_wrf = sbt(es, "wrf", [128, 8, NE], F32)
                            I(DSP, lambda h: h.dma_start(out=wrf[:], in_=wrt.rearrange("(k p) e -> p k e", p=128)), writes=[r_wrf])
                            xn2f, r_xn2f = sbt(es, "xn2f", [128, D], F32)
                            xn2fT, r_xn2fT = sbt(es, "xn2fT", [128, 8, 128], F32)
                            rt, r_rt = sbt(es, "rt", [128, 64], F32)

                        def post_attn(t_out, nsub, P_, x_loader, set_i, sub_base):
                            stp = [dict() for _ in range(nsub)]

                            pdef = []

                            def PDI(*a, **k):
                                pdef.append((a, k))

                            def pflush():
                                for a, k in pdef:
                                    I(*a, **k)
                                pdef.clear()

                            def P0_(sub):
                                stp[sub]['xb'] = x_loader(sub)

                            def P1_(sub):
                                xb, r_xb = stp[sub]['xb']
                                x1, r_x1 = x1b.next()
                                for half in range(2):
                                    py, r_py = pY.next()
                                    for m in range(8):
                                        I(PE, lambda h: h.matmul(py[:P_, :], lhsT=OTs[:, m, sub * 128: sub * 128 + P_],
                                                                 rhs=wo[:, m, half * 512:(half + 1) * 512], start=(m == 0), stop=(m == 7)),
                                          reads=[r_OTs, r_wo], writes=[r_py])
                                    cs_ = slice(half * 512, (half + 1) * 512)
                                    I(DVE, lambda h: h.tensor_tensor(out=x1[:P_, cs_], in0=py[:P_, :], in1=AD[set_i][2][0][:P_, cs_], op=ALU.mult),
                                      reads=[r_py, AD[set_i][2][1]], writes=[r_x1])
                                I(POOL, lambda h: h.tensor_tensor(out=x1[:P_, :], in0=x1[:P_, :], in1=xb[:P_, :], op=ALU.add),
                                  reads=[r_x1, r_xb], writes=[r_x1])
                                if l == 0:
                                    rr = t_out * TS + sub * 128 if t_out < NTILE else NT
                                    PDI(DSP, lambda h: h.dma_start(out=X1[rr:rr + P_, :], in_=x1[:P_, :]), reads=[r_x1], writes=[r_X1[t_out]])
                                else:
                                    rr = t_out * TS + sub * 128 if t_out < 8 else NT // 2
                                    PDI(DSP, lambda h: h.dma_start(out=X3[rr:rr + P_, :], in_=x1[:P_, :]), reads=[r_x1], writes=[r_X3[t_out]])
                                stp[sub].update(x1=(x1, r_x1), rr=rr)

                            def P2_(sub):
                                x1, r_x1 = stp[sub]['x1']
                                rr = stp[sub]['rr']
                                xnT, r_xnT = norm_to_T(nb, x1[:P_, :], r_x1, AD[set_i][4], AD[set_i][3], P_)
                                PDI(DSP, lambda h: h.dma_start(out=XN2T[l][:, :, rr:rr + P_].rearrange("m p t -> p m t"), in_=xnT[:, :, :P_]),
                                  reads=[r_xnT], writes=[r_XN2T[l][t_out]])
                                if l == 1:
                                    sg = sub_base + sub
                                    ssl = nb[1].items[(nb[1].i - 1) % 2]
                                    I(DVE, lambda h: h.scalar_tensor_tensor(out=xn2f[:P_, :], in0=x1[:P_, :], scalar=ssl[0][:P_, 2:3],
                                                                            in1=AD[set_i][4][0][:P_, :], op0=ALU.mult, op1=ALU.mult),
                                      reads=[r_x1, ssl[1], AD[set_i][4][1]], writes=[r_xn2f])
                                    I(POOL, lambda h: h.tensor_tensor(out=xn2f[:P_, :], in0=xn2f[:P_, :], in1=AD[set_i][3][0][:P_, :], op=ALU.add),
                                      reads=[r_xn2f, AD[set_i][3][1]], writes=[r_xn2f])
                                    for hf in range(2):
                                        py, r_py = pY.next()
                                        for k in range(4):
                                            kk = hf * 4 + k
                                            I(PE, lambda h: h.transpose(out=py[:, k * 128:k * 128 + P_], in_=xn2f[:P_, kk * 128:(kk + 1) * 128],
                                                                        identity=identf[:P_, :P_]),
                                              reads=[r_xn2f, r_identf], writes=[r_py])
                                        evac(xn2fT[:, hf * 4:(hf + 1) * 4, :P_], py[:, :].rearrange("p (k t) -> p k t", t=128)[:, :, :P_],
                                             [r_py], [r_xn2fT])
                                    py, r_py = pY.next()
                                    for k in range(8):
                                        I(PE, lambda h: h.matmul(py[:P_, 0:NE], lhsT=xn2fT[:, k, :P_], rhs=wrf[:, k, :],
                                                                 start=(k == 0), stop=(k == 7)),
                                          reads=[r_xn2fT, r_wrf], writes=[r_py])
                                    I(DVE, lambda h: h.tensor_copy(out=rt[:P_, 0:8], in_=py[:P_, 0:NE]), reads=[r_py], writes=[r_rt])
                                    I(DVE, lambda h: h.tensor_reduce(out=rt[:P_, 32:33], in_=rt[:P_, 0:8], axis=AX.X, op=ALU.max),
                                      reads=[r_rt], writes=[r_rt])
                                    I(DVE, lambda h: h.tensor_scalar(out=rt[:P_, 8:16], in0=rt[:P_, 0:8], scalar1=rt[:P_, 32:33], scalar2=None,
                                                                     op0=ALU.is_equal), reads=[r_rt], writes=[r_rt])
                                    I(DVE, lambda h: h.scalar_tensor_tensor(out=rt[:P_, 16:24], in0=rt[:P_, 8:16], scalar=-1e30, in1=rt[:P_, 0:8],
                                                                            op0=ALU.mult, op1=ALU.add), reads=[r_rt], writes=[r_rt])
                                    I(DVE, lambda h: h.tensor_reduce(out=rt[:P_, 33:34], in_=rt[:P_, 16:24], axis=AX.X, op=ALU.max),
                                      reads=[r_rt], writes=[r_rt])
                                    I(DVE, lambda h: h.tensor_scalar(out=rt[:P_, 24:32], in0=rt[:P_, 16:24], scalar1=rt[:P_, 33:34], scalar2=None,
                                                                     op0=ALU.is_equal), reads=[r_rt], writes=[r_rt])
                                    I(DVE, lambda h: h.tensor_tensor(out=rt[:P_, 34:35], in0=rt[:P_, 32:33], in1=rt[:P_, 33:34], op=ALU.subtract),
                                      reads=[r_rt], writes=[r_rt])
                                    I(ACT, lambda h: h.activation(out=rt[:P_, 34:35], in_=rt[:P_, 34:35], func=AF.Exp), reads=[r_rt], writes=[r_rt])
                                    I(DVE, lambda h: h.tensor_scalar(out=rt[:P_, 34:35], in0=rt[:P_, 34:35], scalar1=1.0, scalar2=None, op0=ALU.add),
                                      reads=[r_rt], writes=[r_rt])
                                    I(DVE, lambda h: h.reciprocal(out=rt[:P_, 35:36], in_=rt[:P_, 34:35]), reads=[r_rt], writes=[r_rt])
                                    I(DVE, lambda h: h.tensor_scalar(out=rt[:P_, 34:35], in0=rt[:P_, 35:36], scalar1=-1.0, scalar2=1.0,
                                                                     op0=ALU.mult, op1=ALU.add), reads=[r_rt], writes=[r_rt])
                                    I(DVE, lambda h: h.tensor_scalar(out=rt[:P_, 8:16], in0=rt[:P_, 8:16], scalar1=rt[:P_, 34:35], scalar2=None,
                                                                     op0=ALU.mult), reads=[r_rt], writes=[r_rt])
                                    I(DVE, lambda h: h.scalar_tensor_tensor(out=gates[:P_, sg, :], in0=rt[:P_, 24:32], scalar=rt[:P_, 35:36],
                                                                            in1=rt[:P_, 8:16], op0=ALU.mult, op1=ALU.add),
                                      reads=[r_rt], writes=[r_gates])

                            la_ = 2 if l == 0 else 1
                            for q_ in range(min(la_, nsub)):
                                P0_(q_)
                            for q_ in range(nsub + 1):
                                pflush()
                                if q_ + la_ < nsub:
                                    P0_(q_ + la_)
                                if q_ < nsub:
                                    P1_(q_)
                                if q_ - 1 >= 0:
                                    P2_(q_ - 1)
                            pflush()

                        if l == 0:
                            with ExitStack() as e2:
                                wrl, r_wrl = sbt(e2, "wrl", [128, 16, WRELW], BF16)
                                for hh in range(0, 16, 4):
                                    I(DPOOL, lambda h: h.dma_start(out=wrl[:, hh:hh + 4, :], in_=wrel[hh:hh + 4].rearrange("h p w -> p h w")),
                                      writes=[r_wrl])
                                PTb = Rot([sbt(e2, f"PTb{i}", [128, TS], BF16) for i in range(3)])
                                rec, r_rec = sbt(e2, "rec", [128, TS], F32)
                                e3 = e2.enter_context(ExitStack())
                                qTb = Rot([sbt(e3, f"qTb{i}", [128, 8, TS], BF16) for i in range(1)])
                                KTr = [sbt(e3, f"KTr{i}", [128, 8, TS], BF16) for i in range(2)]
                                Vr = [sbt(e3, f"Vr{i}", [128, 4, D], BF16) for i in range(2)]
                                pS = pY
                                pO = Rot([pst(e2, f"pO{i}", [128, 512], F32) for i in range(2)])
                                pR = Rot([pst(e2, f"pR{i}", [128, 512], F32) for i in range(2)])

                                def band_run(entries):
                                    flat = []
                                    for (hd, q_ap_fn, units, ncols, out_cols) in entries:
                                        nu = len(units)
                                        for i, u in enumerate(units):
                                            flat.append((hd, q_ap_fn, ncols, out_cols, i == 0, i == nu - 1, u))
                                    st = {}

                                    def A(k):
                                        hd, q_ap_fn, ncols, out_cols, first, last, (kT_fn, v_ap, nk, woff, lo, hi, ms, rds) = flat[k]
                                        r0 = (hd % 2) * 64
                                        ps, r_ps = pS.next()
                                        q_ap, r_q = q_ap_fn(r0, lo, hi)
                                        I(PE, lambda h: h.matmul(ps[:nk, lo:hi], lhsT=kT_fn(r0), rhs=q_ap, start=True, stop=False),
                                          reads=rds + [r_q], writes=[r_ps])
                                        I(PE, lambda h: h.matmul(ps[:nk, lo:hi], lhsT=identb[:nk, :nk], rhs=wrl[:nk, hd, woff + lo:woff + hi],
                                                                 start=False, stop=True),
                                          reads=[r_identb, r_wrl], writes=[r_ps])
                                        st[k] = (ps, r_ps)

                                    def B(k):
                                        hd, q_ap_fn, ncols, out_cols, first, last, (kT_fn, v_ap, nk, woff, lo, hi, ms, rds) = flat[k]
                                        ps, r_ps = st[k]
                                        pt, r_pt = PTb.next()
                                        I(ACT, lambda h: h.activation(out=pt[:nk, lo:hi], in_=ps[:nk, lo:hi], func=AF.Exp), reads=[r_ps], writes=[r_pt])
                                        if ms is not None:
                                            rows, c0 = ms
                                            I(POOL, lambda h: h.memset(pt[rows:rows + 64, c0:c0 + 64], 0.0), writes=[r_pt])
                                        st[k] = (pt, r_pt)

                                    def C(k):
                                        hd, q_ap_fn, ncols, out_cols, first, last, (kT_fn, v_ap, nk, woff, lo, hi, ms, rds) = flat[k]
                                        m, r0 = hd // 2, (hd % 2) * 64
                                        pt, r_pt = st.pop(k)
                                        if first:
                                            st["po"] = pO.next()
                                            st["pr"] = pR.next()
                                        po, r_po = st["po"]
                                        pr, r_pr = st["pr"]
                                        I(PE, lambda h: h.matmul(po[:, lo:hi], lhsT=v_ap, rhs=pt[:nk, lo:hi], start=first, stop=last),
                                          reads=rds + [r_pt], writes=[r_po])
                                        I(PE, lambda h: h.matmul(pr[:, lo:hi], lhsT=onesb[:nk, :], rhs=pt[:nk, lo:hi], start=first, stop=last),
                                          reads=[r_onesb, r_pt], writes=[r_pr])
                                        if last:
                                            I(DVE, lambda h: h.reciprocal(out=rec[r0:r0 + 64, :ncols], in_=pr[r0:r0 + 64, :ncols]),
                                              reads=[r_pr], writes=[r_rec])
                                            I(DVE, lambda h: h.tensor_tensor(out=OTs[r0:r0 + 64, m, out_cols], in0=po[r0:r0 + 64, :ncols],
                                                                             in1=rec[r0:r0 + 64, :ncols], op=ALU.mult),
                                              reads=[r_po, r_rec], writes=[r_OTs])

                                    n = len(flat)
                                    A(0)
                                    for k in range(n):
                                        if k + 1 < n:
                                            A(k + 1)
                                        B(k)
                                        C(k)

                                COLS = [(0, 128), (0, 256), (0, 384), (0, 512), (0, 512), (128, 512), (256, 512), (384, 512)]
                                for t in range(NTILE):
                                    qT, r_qT = qTb.next()
                                    I(DSP, lambda h: h.dma_start(out=qT[:], in_=QT[0][:, :, t * TS:(t + 1) * TS].rearrange("m p t -> p m t")),
                                      reads=[r_QT[0][t]], writes=[r_qT])
                                    kc, r_kc = KTr[t % 2]
                                    vc, r_vc = Vr[t % 2]
                                    I(DSP, lambda h: h.dma_start(out=kc[:], in_=KT[0][:, :, t * TS:(t + 1) * TS].rearrange("m p t -> p m t")),
                                      reads=[r_KT[0][t]], writes=[r_kc])
                                    I(DSP, lambda h: h.dma_start(out=vc[:], in_=VB[0][t * TS:(t + 1) * TS, :].rearrange("(k p) f -> p k f", p=128)),
                                      reads=[r_VB[0][t]], writes=[r_vc])
                                    entries = []
                                    for hd in range(16):
                                        m = hd // 2
                                        units = []
                                        order = [4, 5, 6, 7] + ([0, 1, 2, 3] if t > 0 else [])
                                        for kr in order:
                                            tt = t if kr >= 4 else t - 1
                                            ktl = kr % 4
                                            kb, r_kb = KTr[tt % 2]
                                            vb_, r_vb_ = Vr[tt % 2]
                                            lo, hi = COLS[kr]
                                            ms = (0, (2 * kr + 1) * 64) if kr <= 3 else (64, (2 * kr - 8) * 64)
                                            units.append((
                                                (lambda r0, kb=kb, ktl=ktl, m=m: kb[r0:r0 + 64, m, ktl * 128:(ktl + 1) * 128]),
                                                vb_[:, ktl, m * 128:(m + 1) * 128], 128, 639 - 128 * kr, lo, hi, ms, [r_kb, r_vb_]))
                                        entries.append((hd, (lambda r0, lo, hi, qT=qT, m=m, r_qT=r_qT: (qT[r0:r0 + 64, m, lo:hi], r_qT)),
                                                        units, TS, slice(0, TS)))
                                    band_run(entries)

                                    def xload(sub, t=t):
                                        xb, r_xb = xbuf.next()
                                        I(DSP, lambda h: h.dma_start(out=xb[:, :], in_=xp[t * TS + sub * 128: t * TS + (sub + 1) * 128, :]), writes=[r_xb])
                                        return xb, r_xb
                                    post_attn(t, 4, 128, xload, 0, 0)

                                S.barrier()
                                e3.close()
                                ckb, r_ckb = sbt(e2, "ckb", [128, D], BF16)
                                cKT, r_cKT = sbt(e2, "cKT", [128, 8, 512], BF16)
                                cV, r_cV = sbt(e2, "cV", [128, 4, D], BF16)
                                nKT, r_nKT = sbt(e2, "nKT", [128, 8, 32], BF16)
                                nV, r_nV = sbt(e2, "nV", [32, D], BF16)
                                qTs, r_qTs = sbt(e2, "qTs", [128, 8, 32], BF16)
                                pT2 = nb[4]
                                for s in range(2):
                                    c0 = NT + s * 32
                                    for kt in range(4):
                                        I(DPOOL, lambda h: h.dma_start(out=ckb[:], in_=cak[s, kt * 128:(kt + 1) * 128, :]), writes=[r_ckb])
                                        pt, r_pt = pT2.next()
                                        for m in range(8):
                                            I(PE, lambda h: h.transpose(out=pt[:, m, :], in_=ckb[:, m * 128:(m + 1) * 128], identity=identb[:]),
                                              reads=[r_ckb, r_identb], writes=[r_pt])
                                        evac(cKT[:, :, kt * 128:(kt + 1) * 128], pt[:], [r_pt], [r_cKT])
                                    I(DPOOL, lambda h: h.dma_start(out=cV[:], in_=cav[s].rearrange("(k p) f -> p k f", p=128)), writes=[r_cV])
                                    I(DSP, lambda h: h.dma_start(out=nKT[:], in_=KT[0][:, :, c0:c0 + 32].rearrange("m p t -> p m t")),
                                      reads=[r_KT[0][16]], writes=[r_nKT])
                                    I(DSP, lambda h: h.dma_start(out=nV[:], in_=VB[0][c0:c0 + 32, :]), reads=[r_VB[0][16]], writes=[r_nV])
                                    I(DSP, lambda h: h.dma_start(out=qTs[:], in_=QT[0][:, :, c0:c0 + 32].rearrange("m p t -> p m t")),
                                      reads=[r_QT[0][16]], writes=[r_qTs])
                                    entries = []
                                    for hd in range(16):
                                        m = hd // 2
                                        units = []
                                        for kt in range(4):
                                            units.append(((lambda r0, kt=kt, m=m: cKT[r0:r0 + 64, m, kt * 128:(kt + 1) * 128]),
                                                          cV[:, kt, m * 128:(m + 1) * 128], 128, 639 - 128 * kt, 0, 32, None, [r_cKT, r_cV]))
                                        units.append(((lambda r0, m=m: nKT[r0:r0 + 64, m, :]), nV[:, m * 128:(m + 1) * 128], 32, 127, 0, 32, None,
                                                      [r_nKT, r_nV]))
                                        entries.append((hd, (lambda r0, lo, hi, m=m: (qTs[r0:r0 + 64, m, lo:hi], r_qTs)), units, 32,
                                                        slice(s * 32, (s + 1) * 32)))
                                    band_run(entries)

                                def xload_s(sub):
                                    xb, r_xb = xbuf.next()
                                    I(DSP, lambda h: h.dma_start(out=xb[:SMP, :], in_=xs[:, :]), writes=[r_xb])
                                    return xb, r_xb
                                post_attn(16, 1, SMP, xload_s, 1, 0)
                                S.barrier()
                        else:
                            with ExitStack() as e2:
                                msb, r_msb = sbt(e2, "msb", [128, 8, 512], BF16)
                                msmp, r_msmp = sbt(e2, "msmp", [32, 32], BF16)
                                I(DPOOL, lambda h: h.dma_start(out=msb[:], in_=msb_d), writes=[r_msb])
                                I(DPOOL, lambda h: h.dma_start(out=msmp[:], in_=msmp_d), writes=[r_msmp])
                                qTa, r_qTa = sbt(e2, "qTa", [128, 8, TS], BF16)
                                qTbb, r_qTbb = sbt(e2, "qTbb", [128, 8, TS], BF16)
                                qTo, r_qTo = qTbb, r_qTbb
                                r_kmc = [Res() for _ in range(8)]
                                r_vmc = [Res() for _ in range(8)]
                                KTm = Rot([sbt(e2, f"KTm{i}", [128, NT], BF16) for i in range(1)])
                                Vm = Rot([sbt(e2, f"Vm{i}", [128, NT // 128, 128], BF16) for i in range(1)])
                                eb = Rot([sbt(e2, f"eb{i}", [128, TS], F32) for i in range(2)])
                                Lb = Rot([sbt(e2, f"Lb{i}", [128, TS], BF16) for i in range(4)])
                                ab = Rot([sbt(e2, f"ab{i}", [128, TS], BF16) for i in range(4)])
                                Lacc32s = [sbt(e2, f"Lacc32_{i}", [128, TS], F32) for i in range(2)]
                                Lacc16s = [Rot([sbt(e2, f"Lacc16_{k}_{i}", [128, TS], BF16) for i in range(2)]) for k in range(2)]
                                pZ = pY
                                pO = Rot([pst(e2, f"pO{i}", [128, 512], F32) for i in range(2)])
                                pF, r_pF = pst(e2, "pF", [128, 512], F32)

                                def fill(n):
                                    for _ in range(n):
                                        I(PE, lambda h: h.matmul(pF[:, :], lhsT=identb[:, :], rhs=msb[:, 0, :], start=True, stop=True),
                                          reads=[r_identb, r_msb], writes=[r_pF])

                                def sb_group(streams, nq):
                                    ns_ = len(streams)
                                    nu = len(streams[0][3])
                                    S_ = []
                                    for si, (hd, q_ap, r_q, units, out_cols) in enumerate(streams):
                                        la, r_la = Lacc32s[si]
                                        I(POOL, lambda h: h.memset(la[:, :nq], 0.0), writes=[r_la])
                                        S_.append(dict(hd=hd, q=q_ap, rq=r_q, u=units, oc=out_cols, po=pO.next(), la=(la, r_la),
                                                       l16=None, pz={}, L={}, a={}, l16m={}))

                                    def A(d, i):
                                        kT_ap, v_ap, nk, mask, rds = d["u"][i]
                                        pz, r_pz = pZ.next()
                                        I(PE, lambda h: h.matmul(pz[:nk, :nq], lhsT=kT_ap, rhs=d["q"], start=True, stop=False, skip_group_check=True),
                                          reads=rds + [d["rq"]], writes=[r_pz])
                                        d["pz"][i] = (pz, r_pz)

                                    def B(d, i, si):
                                        kT_ap, v_ap, nk, mask, rds = d["u"][i]
                                        pz, r_pz = d["pz"][i]
                                        e_, r_e = eb.next()
                                        L_, r_L = Lb.next()
                                        I(ACT, lambda h: h.activation(out=e_[:nk, :nq], in_=pz[:nk, :nq], func=AF.Exp), reads=[r_pz], writes=[r_e])
                                        I(ACT, lambda h: h.activation(out=L_[:nk, :nq], in_=e_[:nk, :nq], func=AF.Ln, bias=1.0, scale=1.0),
                                          reads=[r_e], writes=[r_L])
                                        if mask is not None:
                                            I(DVE, lambda h: h.tensor_tensor(out=L_[:nk, :nq], in0=L_[:nk, :nq], in1=mask[0], op=ALU.mult),
                                              reads=[r_L, mask[1]], writes=[r_L])
                                        d["L"][i] = (L_, r_L)
                                        if i < nu - 1:
                                            la, r_la = d["la"]
                                            I(DVE, lambda h: h.tensor_tensor(out=la[:nk, :nq], in0=la[:nk, :nq], in1=L_[:nk, :nq], op=ALU.add),
                                              reads=[r_la, r_L], writes=[r_la])
                                            l16, r_l16 = Lacc16s[si].next()
                                            ceng = DVE
                                            I(ceng, lambda h: h.tensor_copy(out=l16[:, :nq], in_=la[:, :nq]), reads=[r_la], writes=[r_l16])
                                            d["l16m"][i] = (l16, r_l16)

                                    def C(d, i):
                                        kT_ap, v_ap, nk, mask, rds = d["u"][i]
                                        pz, r_pz = d["pz"][i]
                                        L_, r_L = d["L"].pop(i)
                                        I(PE, lambda h: h.matmul(pz[:nk, :nq], lhsT=negtri[:nk, :nk], rhs=L_[:nk, :nq], start=False, stop=(i == 0),
                                                                 skip_group_check=True),
                                          reads=[r_negtri, r_L], writes=[r_pz])
                                        if i > 0:
                                            l16, r_l16 = d["l16m"].pop(i - 1)
                                            I(PE, lambda h: h.matmul(pz[:nk, :nq], lhsT=negones[:, :nk], rhs=l16[:, :nq], start=False, stop=True,
                                                                     skip_group_check=True),
                                              reads=[r_negones, r_l16], writes=[r_pz])

                                    def Dd(d, i):
                                        kT_ap, v_ap, nk, mask, rds = d["u"][i]
                                        pz, r_pz = d["pz"].pop(i)
                                        a_, r_a = ab.next()
                                        I(ACT, lambda h: h.activation(out=a_[:nk, :nq], in_=pz[:nk, :nq], func=AF.Exp), reads=[r_pz], writes=[r_a])
                                        if mask is not None:
                                            I(DVE, lambda h: h.tensor_tensor(out=a_[:nk, :nq], in0=a_[:nk, :nq], in1=mask[0], op=ALU.mult),
                                              reads=[r_a, mask[1]], writes=[r_a])
                                        d["a"][i] = (a_, r_a)

                                    def E(d, i):
                                        kT_ap, v_ap, nk, mask, rds = d["u"][i]
                                        a_, r_a = d["a"].pop(i)
                                        po, r_po = d["po"]
                                        I(PE, lambda h: h.matmul(po[:, :nq], lhsT=v_ap, rhs=a_[:nk, :nq], start=(i == 0), stop=(i == nu - 1)),
                                          reads=rds + [r_a], writes=[r_po])

                                    for d in S_:
                                        A(d, 0)
                                    for si, d in enumerate(S_):
                                        B(d, 0, si)
                                    def LAev(d, i, si):
                                        la, r_la = d["la"]
                                        l16, r_l16 = Lacc16s[si].next()
                                        I(DVE, lambda h: h.tensor_copy(out=l16[:, :nq], in_=la[:, :nq]), reads=[r_la], writes=[r_l16])
                                        d["l16m"][i] = (l16, r_l16)

                                    nf = 1 if nq == TS else 0
                                    for i in range(nu):
                                        for si, d in enumerate(S_):
                                            fill(2 * nf)
                                            C(d, i)
                                            if i + 1 < nu:
                                                A(d, i + 1)
                                            Dd(d, i)
                                            if i + 1 < nu:
                                                B(d, i + 1, si)
                                            fill(nf)
                                            E(d, i)
                                    for d in S_:
                                        m, r0 = d["hd"] // 2, (d["hd"] % 2) * 64
                                        po, r_po = d["po"]
                                        evac(OTs[r0:r0 + 64, m, d["oc"]], po[r0:r0 + 64, :nq], [r_po], [r_OTs])

                                for j in range(8):
                                    ta, tb = 2 * j, 2 * j + 1
                                    I(DSP, lambda h: h.dma_start(out=qTa[:], in_=QT[1][:, :, ta * TS:(ta + 1) * TS].rearrange("m p t -> p m t")),
                                      reads=[r_QT[1][ta]], writes=[r_qTa])
                                    I(DSP, lambda h: h.dma_start(out=qTbb[:], in_=QT[1][:, :, tb * TS:(tb + 1) * TS].rearrange("m p t -> p m t")),
                                      reads=[r_QT[1][tb]], writes=[r_qTbb])
                                    I(DVE, lambda h: h.tensor_scalar(out=qTa[:], in0=qTa[:], scalar1=pm[:, 0:1], scalar2=None, op0=ALU.mult),
                                      reads=[r_qTa, r_pm], writes=[r_qTa])
                                    I(DVE, lambda h: h.scalar_tensor_tensor(out=qTo[:], in0=qTbb[:], scalar=pm[:, 1:2], in1=qTa[:],
                                                                            op0=ALU.mult, op1=ALU.add),
                                      reads=[r_qTa, r_pm], writes=[r_qTo])
                                    nk_all = (2 * j + 2) * TS
                                    nkt = nk_all // 128
                                    for m in range(8):
                                        km, _ = KTm.next()
                                        vm, _ = Vm.next()
                                        nch = nk_all // 1024
                                        for c in range(nch - 1, -1, -1):
                                            k0, k1 = c * 1024, (c + 1) * 1024
                                            I(DSP, lambda h: h.dma_start(out=km[:, k0:k1], in_=KT[1][m, :, k0:k1]),
                                              reads=[r_KT[1][2 * c], r_KT[1][2 * c + 1]], writes=[r_kmc[c]])
                                            I(DSP, lambda h: h.dma_start(out=vm[:, k0 // 128:k1 // 128, :], in_=VBm[m, :, k0 // 128:k1 // 128, :]),
                                              reads=[r_VB[1][2 * c], r_VB[1][2 * c + 1]], writes=[r_vmc[c]])
                                        streams = []
                                        for hh in range(2):
                                            hd = 2 * m + hh
                                            r0 = hh * 64
                                            units = []
                                            for kt in range(nkt - 1, -1, -1):
                                                krel = kt - 8 * j
                                                mask = (msb[:, krel, :], r_msb) if krel >= 0 else None
                                                units.append((km[r0:r0 + 64, kt * 128:(kt + 1) * 128], vm[:, kt, :], 128, mask,
                                                              [r_kmc[kt // 8], r_vmc[kt // 8]]))
                                            streams.append((hd, qTo[r0:r0 + 64, m, :], r_qTo, units, slice(0, TS)))
                                        sb_group(streams, TS)

                                    def xload(sub, j=j):
                                        xa, r_xa = xbuf.next()
                                        xb_, r_xb_ = xbuf.next()
                                        ra = (2 * j) * TS + sub * 128
                                        rb = (2 * j + 1) * TS + sub * 128
                                        I(DSP, lambda h: h.dma_start(out=xa[:, :], in_=X2[ra:ra + 128, :]), reads=[r_X2[2 * j]], writes=[r_xa])
                                        I(DSP, lambda h: h.dma_start(out=xb_[:, :], in_=X2[rb:rb + 128, :]), reads=[r_X2[2 * j + 1]], writes=[r_xb_])
                                        I(DVE, lambda h: h.tensor_scalar(out=xa[:, :], in0=xa[:, :], scalar1=pm[:, 0:1], scalar2=None, op0=ALU.mult),
                                          reads=[r_xa, r_pm], writes=[r_xa])
                                        I(DVE, lambda h: h.scalar_tensor_tensor(out=xb_[:, :], in0=xb_[:, :], scalar=pm[:, 1:2], in1=xa[:, :],
                                                                                op0=ALU.mult, op1=ALU.add),
                                          reads=[r_xa, r_xb_, r_pm], writes=[r_xb_])
                                        return xb_, r_xb_
                                    post_attn(j, 4, 128, xload, 0, j * 4)

                                ckb, r_ckb = sbt(e2, "ckb1", [128, D], BF16)
                                ckT, r_ckT = sbt(e2, "ckT1", [128, 8, 128], BF16)
                                pT2 = nb[4]
                                for s in range(2):
                                    for kt in range(32):
                                        I(DPOOL, lambda h: h.dma_start(out=ckb[:], in_=cbk[s, kt * 128:(kt + 1) * 128, :]), writes=[r_ckb])
                                        pt, r_pt = pT2.next()
                                        for m in range(8):
                                            I(PE, lambda h: h.transpose(out=pt[:, m, :], in_=ckb[:, m * 128:(m + 1) * 128], identity=identb[:]),
                                              reads=[r_ckb, r_identb], writes=[r_pt])
                                        evac(ckT[:], pt[:], [r_pt], [r_ckT])
                                        I(DSP, lambda h: h.dma_start(out=CKT[s, :, :, kt * 128:(kt + 1) * 128].rearrange("m p t -> p m t"), in_=ckT[:]),
                                          reads=[r_ckT], writes=[r_CKT[s]])
                                nKT, r_nKT = sbt(e2, "nKT1", [128, 8, 32], BF16)
                                nV, r_nV = sbt(e2, "nV1", [32, D], BF16)
                                qTs, r_qTs = sbt(e2, "qTs1", [128, 8, 32], BF16)
                                for s in range(2):
                                    c0 = NT + s * 32
                                    I(DSP, lambda h: h.dma_start(out=nKT[:], in_=KT[1][:, :, c0:c0 + 32].rearrange("m p t -> p m t")),
                                      reads=[r_KT[1][16]], writes=[r_nKT])
                                    I(DSP, lambda h: h.dma_start(out=nV[:, :].rearrange("p (m f) -> p m f", f=128),
                                                                 in_=VBm[:, s * 32:(s + 1) * 32, NT // 128, :].rearrange("m p f -> p m f")),
                                      reads=[r_VB[1][16]], writes=[r_nV])
                                    I(DSP, lambda h: h.dma_start(out=qTs[:], in_=QT[1][:, :, c0:c0 + 32].rearrange("m p t -> p m t")),
                                      reads=[r_QT[1][16]], writes=[r_qTs])
                                    for m in range(8):
                                        km, _ = KTm.next()
                                        vm, _ = Vm.next()
                                        for c in range(3, -1, -1):
                                            k0, k1 = c * 1024, (c + 1) * 1024
                                            I(DSP, lambda h: h.dma_start(out=km[:, k0:k1], in_=CKT[s, m, :, k0:k1]), reads=[r_CKT[s]], writes=[r_kmc[c]])
                                            I(DPOOL, lambda h: h.dma_start(
                                                out=vm[:, k0 // 128:k1 // 128, :],
                                                in_=cbv[s, k0:k1, m * 128:(m + 1) * 128].rearrange("(k p) f -> p k f", p=128)),
                                              writes=[r_vmc[c]])
                                        streams = []
                                        for hh in range(2):
                                            hd = 2 * m + hh
                                            r0 = hh * 64
                                            units = [(nKT[r0:r0 + 64, m, :], nV[:, m * 128:(m + 1) * 128], 32, (msmp[:, :], r_msmp), [r_nKT, r_nV])]
                                            for kt in range(31, -1, -1):
                                                units.append((km[r0:r0 + 64, kt * 128:(kt + 1) * 128], vm[:, kt, :], 128, None,
                                                              [r_kmc[kt // 8], r_vmc[kt // 8]]))
                                            streams.append((hd, qTs[r0:r0 + 64, m, :], r_qTs, units, slice(s * 32, (s + 1) * 32)))
                                        sb_group(streams, 32)

                                def xload_s(sub):
                                    xb, r_xb = xbuf.next()
                                    I(DSP, lambda h: h.dma_start(out=xb[:SMP, :], in_=X2[NT:NT + SMP, :]), reads=[r_X2[16]], writes=[r_xb])
                                    return xb, r_xb
                                post_attn(8, 1, SMP, xload_s, 1, 32)
                                S.barrier()
                    if stop_after == ("A2", l):
                        break

                with ExitStack() as es:
                    n_exp = 1 if l == 0 else NE
                    wgs, wus, wds = (wgd, wud, wdd) if l == 0 else (wge, wue, wde)
                    ngroups = 4 if l == 0 else 2
                    x_res, r_xres = (X1, r_X1) if l == 0 else (X3, r_X3)
                    samp_row = NT if l == 0 else NT // 2
                    xg, r_xg = sbt(es, "xg", [128, 8, 2048 + SMP], BF16)
                    acc, r_acc = sbt(es, "acc", [128, 17, D], F32)
                    wgb = Rot([sbt(es, f"wgb{i}", [128, 8, 512], BF16) for i in range(2)])
                    wub = Rot([sbt(es, f"wub{i}", [128, 8, 512], BF16) for i in range(2)])
                    wdb = Rot([sbt(es, f"wdb{i}", [128, 4, D], BF16) for i in range(2)])
                    hTb = Rot([sbt(es, f"hTb{i}", [128, 4, TS], BF16) for i in range(2)])
                    sgb = Rot([sbt(es, f"sgb{i}", [128, TS], F32) for i in range(2)])
                    xfb = Rot([sbt(es, f"xfb{i}", [128, D], F32) for i in range(2)])
                    pG = Rot([pst(es, f"pG{i}", [128, 512], F32) for i in range(2)])
                    pU = Rot([pst(es, f"pU{i}", [128, 512], F32) for i in range(2)])
                    pYf = Rot([pst(es, f"pYf{i}", [128, 512], F32) for i in range(3)])
                    for g in range(ngroups):
                        last = (g == ngroups - 1)
                        tiles = [(g * 4 + i, i * TS, TS, 4, 128) for i in range(4)]
                        if last:
                            tiles.append((16 if l == 0 else 8, 2048, SMP, 1, SMP))
                        ncol = 2048 + (SMP if last else 0)
                        rds = [r_XN2T[l][tid] for (tid, _, _, _, _) in tiles]
                        I(DSP, lambda h: h.dma_start(out=xg[:, :, 0:2048],
                                                     in_=XN2T[l][:, :, g * 2048:(g + 1) * 2048].rearrange("m p t -> p m t")),
                          reads=rds[:4], writes=[r_xg])
                        if last:
                            I(DSP, lambda h: h.dma_start(out=xg[:, :, 2048:2048 + SMP],
                                                         in_=XN2T[l][:, :, samp_row:samp_row + SMP].rearrange("m p t -> p m t")),
                              reads=rds[4:], writes=[r_xg])
                        first_acc = True
                        for e in range(n_exp):
                            for fc in range(NFC):
                                wg_, r_wg = wgb.next()
                                wu_, r_wu = wub.next()
                                wd_, r_wd = wdb.next()
                                fsl = slice(fc * 512, (fc + 1) * 512)
                                I(DPOOL, lambda h: h.dma_start(out=wg_[:], in_=wgs[e, :, fsl].rearrange("(k p) f -> p k f", p=128)), writes=[r_wg])
                                I(DPOOL, lambda h: h.dma_start(out=wu_[:], in_=wus[e, :, fsl].rearrange("(k p) f -> p k f", p=128)), writes=[r_wu])
                                I(DPOOL, lambda h: h.dma_start(out=wd_[:], in_=wds[e, fsl, :].rearrange("(s p) d -> p s d", p=128)), writes=[r_wd])
                                for ti, (tid, c0, ntok, nsub, P_) in enumerate(tiles):
                                    hT, r_hT = hTb.next()
                                    for fs in range(4):
                                        pg, r_pg = pG.next()
                                        pu, r_pu = pU.next()
                                        for k in range(8):
                                            I(PE, lambda h: h.matmul(pg[:, :ntok], lhsT=wg_[:, k, fs * 128:(fs + 1) * 128], rhs=xg[:, k, c0:c0 + ntok],
                                                                     start=(k == 0), stop=(k == 7)), reads=[r_wg, r_xg], writes=[r_pg])
                                        for k in range(8):
                                            I(PE, lambda h: h.matmul(pu[:, :ntok], lhsT=wu_[:, k, fs * 128:(fs + 1) * 128], rhs=xg[:, k, c0:c0 + ntok],
                                                                     start=(k == 0), stop=(k == 7)), reads=[r_wu, r_xg], writes=[r_pu])
                                        sg_, r_sg = sgb.next()
                                        I(ACT, lambda h: h.activation(out=sg_[:, :ntok], in_=pg[:, :ntok], func=AF.Silu), reads=[r_pg], writes=[r_sg])
                                        I(DVE, lambda h: h.tensor_tensor(out=hT[:, fs, :ntok], in0=pu[:, :ntok], in1=sg_[:, :ntok], op=ALU.mult),
                                          reads=[r_pu, r_sg], writes=[r_hT])
                                    for sub in range(nsub):
                                        sa = ti * 4 + sub
                                        sgl = (g * 16 + sa) if tid < (16 if l == 0 else 8) else 32
                                        for half in range(2):
                                            py, r_py = pYf.next()
                                            for fs in range(4):
                                                I(PE, lambda h: h.matmul(py[:P_, :], lhsT=hT[:, fs, sub * 128: sub * 128 + P_],
                                                                         rhs=wd_[:, fs, half * 512:(half + 1) * 512], start=(fs == 0), stop=(fs == 3)),
                                                  reads=[r_hT, r_wd], writes=[r_py])
                                            dst = acc[:P_, sa, half * 512:(half + 1) * 512]
                                            gsc = gates[:P_, sgl, e:e + 1] if l == 1 else 1.0
                                            grd = [r_gates] if l == 1 else []
                                            if first_acc:
                                                I(DVE, lambda h: h.tensor_scalar(out=dst, in0=py[:P_, :], scalar1=gsc, scalar2=None, op0=ALU.mult),
                                                  reads=[r_py] + grd, writes=[r_acc])
                                            else:
                                                I(DVE, lambda h: h.scalar_tensor_tensor(out=dst, in0=py[:P_, :], scalar=gsc, in1=dst,
                                                                                        op0=ALU.mult, op1=ALU.add),
                                                  reads=[r_py, r_acc] + grd, writes=[r_acc])
                                first_acc = False
                        for ti, (tid, c0, ntok, nsub, P_) in enumerate(tiles):
                            set_i = 1 if P_ == SMP else 0
                            for sub in range(nsub):
                                sa = ti * 4 + sub
                                rr = (tid * TS + sub * 128) if P_ == 128 else samp_row
                                xf, r_xf = xfb.next()
                                I(DSP, lambda h: h.dma_start(out=xf[:P_, :], in_=x_res[rr:rr + P_, :]), reads=[r_xres[tid]], writes=[r_xf])
                                I(POOL, lambda h: h.tensor_tensor(out=acc[:P_, sa, :], in0=acc[:P_, sa, :], in1=G2[set_i][0][:P_, :], op=ALU.mult),
                                  reads=[r_acc, G2[set_i][1]], writes=[r_acc])
                                I(POOL, lambda h: h.tensor_tensor(out=xf[:P_, :], in0=xf[:P_, :], in1=acc[:P_, sa, :], op=ALU.add),
                                  reads=[r_acc, r_xf], writes=[r_xf])
                                if l == 0:
                                    I(DSP, lambda h: h.dma_start(out=X2[rr:rr + P_, :], in_=xf[:P_, :]), reads=[r_xf], writes=[r_X2[tid]])
                                else:
                                    if P_ == 128:
                                        I(DSP, lambda h: h.dma_start(out=yp[rr:rr + P_, :], in_=xf[:P_, :]), reads=[r_xf], writes=[r_out])
                                    else:
                                        I(DSP, lambda h: h.dma_start(out=ys[:, :], in_=xf[:P_, :]), reads=[r_xf], writes=[r_out])
                    S.barrier()

        S.final_wait(S.q_sp, [S.DSP, S.DPOOL, S.PE, S.ACT, S.DVE, S.POOL])
        print(f"[build] instructions={S.ninst} waits={S.nwaits}", flush=True)
    return nc


_NC_CACHE = {}
_DEBUG_HOOK = []


def _host_consts():
    ident = np.eye(128, dtype=np.float32)
    j = np.arange(128)[:, None]
    s = np.arange(128)[None, :]
    negtri = np.where(j >= s, -1.0, 0.0).astype(np.float32)
    msmp = (np.arange(32)[:, None] < np.arange(32)[None, :]).astype(np.float32)
    return ident, negtri, msmp


def _msb_for(p):
    k = np.arange(128)[:, None, None]
    kr = np.arange(8)[None, :, None]
    q = np.arange(512)[None, None, :]
    return ((kr * 128 + k) < (p * 512 + q)).astype(np.float32)


def kernel(x_prompt, x_sample, cache_a_k, cache_a_v, cache_b_k, cache_b_v, c_prompt, c_sample,
           w_qkv, w_o, norm1_g, norm2_g, w_ada, b_ada, q_norm_g, k_norm_g, rel_bias,
           w_gate_d, w_up_d, w_down_d, w_router, w_gate_e, w_up_e, w_down_e):
    f32 = lambda a: np.ascontiguousarray(np.asarray(a, dtype=np.float32))
    x_prompt, x_sample = f32(x_prompt), f32(x_sample)
    cache_a_k, cache_a_v, cache_b_k, cache_b_v = f32(cache_a_k), f32(cache_a_v), f32(cache_b_k), f32(cache_b_v)
    c_prompt, c_sample = f32(c_prompt), f32(c_sample)
    rel_bias = f32(rel_bias)
    kk = np.arange(128)[:, None]
    jj = np.arange(WRELW)[None, :]
    idx = np.clip(jj - 127 - kk, -128, 128) + 128
    wrel = np.ascontiguousarray(rel_bias[0][:, idx])
    ident, negtri, msmp = _host_consts()
    shared = dict(
        w_qkv=f32(w_qkv), w_o=f32(w_o), n1g=f32(norm1_g), n2g=f32(norm2_g), w_ada=f32(w_ada), b_ada=f32(b_ada),
        qng=f32(q_norm_g), kng=f32(k_norm_g), wrel=wrel, wgd=f32(w_gate_d), wud=f32(w_up_d), wdd=f32(w_down_d),
        wrt=f32(w_router)[0], wge=f32(w_gate_e)[0], wue=f32(w_up_e)[0], wde=f32(w_down_e)[0],
        ident=ident, negtri=negtri, msmp=msmp)
    in_maps = []
    for c in range(8):
        b, p = c // 2, c % 2
        m = dict(shared)
        m["xp"] = x_prompt[b]
        m["xs"] = np.ascontiguousarray(x_sample[2 * c:2 * c + 2].reshape(SMP, D))
        m["cak"] = np.ascontiguousarray(cache_a_k[0, 2 * c:2 * c + 2].reshape(2, 512, D))
        m["cav"] = np.ascontiguousarray(cache_a_v[0, 2 * c:2 * c + 2].reshape(2, 512, D))
        m["cbk"] = np.ascontiguousarray(cache_b_k[0, 2 * c:2 * c + 2].reshape(2, 4096, D))
        m["cbv"] = np.ascontiguousarray(cache_b_v[0, 2 * c:2 * c + 2].reshape(2, 4096, D))
        m["cpl"] = np.ascontiguousarray(c_prompt[b].reshape(8, 128).T)
        m["csl"] = np.ascontiguousarray(c_sample[2 * c:2 * c + 2].reshape(2, 8, 128).transpose(2, 1, 0))
        m["msb"] = _msb_for(p)
        pmv = np.zeros((128, 2), np.float32)
        pmv[:, 0] = 1.0 - p
        pmv[:, 1] = float(p)
        m["pm"] = pmv
        in_maps.append(m)
    if _DEBUG_HOOK:
        return _DEBUG_HOOK[0](in_maps)
    if "nc" not in _NC_CACHE:
        _NC_CACHE["nc"] = build_program()
    res = run_bass_kernel_spmd(_NC_CACHE["nc"], in_maps, core_ids=list(range(8)))
    R = res.results
    B, SQ = 4, NT
    y_prompt = np.zeros((B, SQ, D), np.float32)
    y_sample = np.zeros((16, 32, D), np.float32)
    a_k_p = np.zeros((1, B, 512, 16, 64), np.float32)
    a_v_p = np.zeros_like(a_k_p)
    a_k_s = np.zeros((1, 16, 512, 16, 64), np.float32)
    a_v_s = np.zeros_like(a_k_s)
    b_k_p = np.zeros((1, B, SQ, 16, 64), np.float32)
    b_v_p = np.zeros_like(b_k_p)
    b_k_s = np.zeros((1, 16, 32, 16, 64), np.float32)
    b_v_s = np.zeros_like(b_k_s)
    for c in range(8):
        b, p = c // 2, c % 2
        r = R[c]
        ypc = np.asarray(r["yp"]).reshape(8, TS, D)
        for j in range(8):
            t = 2 * j + p
            y_prompt[b, t * TS:(t + 1) * TS] = ypc[j]
        y_sample[2 * c:2 * c + 2] = np.asarray(r["ys"]).reshape(2, 32, D)
        a_k_s[0, 2 * c:2 * c + 2] = np.asarray(r["aks"]).reshape(2, 512, 16, 64)
        a_v_s[0, 2 * c:2 * c + 2] = np.asarray(r["avs"]).reshape(2, 512, 16, 64)
        b_k_s[0, 2 * c:2 * c + 2] = np.asarray(r["bks"]).reshape(2, 32, 16, 64)
        b_v_s[0, 2 * c:2 * c + 2] = np.asarray(r["bvs"]).reshape(2, 32, 16, 64)
        if p == 0:
            a_k_p[0, b] = np.asarray(r["akp"]).reshape(512, 16, 64)
            a_v_p[0, b] = np.asarray(r["avp"]).reshape(512, 16, 64)
        half = slice(p * (SQ // 2), (p + 1) * (SQ // 2))
        b_k_p[0, b, half] = np.asarray(r["bkp"]).reshape(SQ, 16, 64)[half]
        b_v_p[0, b, half] = np.asarray(r["bvp"]).reshape(SQ, 16, 64)[half]
    return (y_prompt, y_sample, a_k_p, a_v_p, a_k_s, a_v_s, b_k_p, b_v_p, b_k_s, b_v_s)
```

```python
import numpy as np
from contextlib import ExitStack
import concourse.bass as bass
import concourse.mybir as mybir
from concourse.bass_utils import run_bass_kernel_spmd

F32 = mybir.dt.float32
BF16 = mybir.dt.bfloat16
AF = mybir.ActivationFunctionType
ALU = mybir.AluOpType
AX = mybir.AxisListType

D = 1024
NT = 8192
TS = 512
NTILE = NT // TS
SMP = 64
NTOT = NT + SMP
FF = 3584
NFC = FF // 512
NE = 8
WRELW = 768
EPS = 1e-6


class Res:
    __slots__ = ("w", "r")

    def __init__(self):
        self.w = None
        self.r = {}


class VEng:
    def __init__(self, name, hw, sems, inc, inorder):
        self.name = name
        self.hw = hw
        self.sems = sems
        self.inc = inc
        self.n = 0
        self.inorder = inorder


class HwQ:
    def __init__(self, name, h):
        self.name = name
        self.h = h
        self.waited = {}
        self.waited_any = {}


class Sched:
    def __init__(self, nc, es, ns_compute=8, ns_dma=24):
        self.nc = nc
        self.nwaits = 0
        self.ninst = 0

        def sems(prefix, n):
            return [es.enter_context(nc.semaphore(f"{prefix}{i}")) for i in range(n)]

        self.q_pe = HwQ("pe", nc.tensor)
        self.q_act = HwQ("act", nc.scalar)
        self.q_dve = HwQ("dve", nc.vector)
        self.q_pool = HwQ("pool", nc.gpsimd)
        self.q_sp = HwQ("sp", nc.sync)
        self.PE = VEng("PE", self.q_pe, sems("spe", ns_compute), 1, True)
        self.ACT = VEng("ACT", self.q_act, sems("sact", ns_compute), 1, True)
        self.DVE = VEng("DVE", self.q_dve, sems("sdve", ns_compute), 1, True)
        self.POOL = VEng("POOL", self.q_pool, sems("spool", ns_compute), 1, True)
        self.DSP = VEng("DSP", self.q_sp, sems("sdsp", ns_dma), 16, False)
        self.DPOOL = VEng("DPOOL", self.q_pool, sems("sdpool", ns_dma), 16, False)

    def _wait(self, hw, ve, seq):
        ns = len(ve.sems)
        slot = seq % ns
        if ve.inorder and hw.waited_any.get(ve.name, -1) >= seq:
            return
        if hw.waited.get((ve.name, slot), -1) >= seq:
            return
        hw.h.wait_ge(ve.sems[slot], (seq // ns + 1) * ve.inc)
        self.nwaits += 1
        hw.waited[(ve.name, slot)] = seq
        if ve.inorder:
            hw.waited_any[ve.name] = max(hw.waited_any.get(ve.name, -1), seq)

    def issue(self, ve, fn, reads=(), writes=()):
        dl = []
        for r in reads:
            if r.w is not None:
                dl.append(r.w)
        for w in writes:
            if w.w is not None:
                dl.append(w.w)
            dl.extend(w.r.values())
        best = {}
        for e, s in dl:
            if e.inorder:
                if best.get(e, (None, -1))[1] < s:
                    best[e] = (e, s)
            else:
                best[(e, s)] = (e, s)
        hw = ve.hw
        if not ve.inorder and ve.n >= len(ve.sems):
            self._wait(hw, ve, ve.n - len(ve.sems))
        for e, s in best.values():
            if e is ve and ve.name == "PE":
                continue
            self._wait(hw, e, s)
        ins = fn(hw.h)
        seq = ve.n
        ve.n += 1
        ns = len(ve.sems)
        ins.then_inc(ve.sems[seq % ns], ve.inc)
        self.ninst += 1
        for r in reads:
            r.r[ve if ve.inorder else (ve, seq)] = (ve, seq)
        for w in writes:
            w.w = (ve, seq)
            w.r = {}
        return seq

    def barrier(self):
        vengs = [self.PE, self.ACT, self.DVE, self.POOL, self.DSP, self.DPOOL]
        for hw in (self.q_pe, self.q_act, self.q_dve, self.q_pool, self.q_sp):
            for ve in vengs:
                if ve.n == 0:
                    continue
                if ve.inorder:
                    if ve.hw is hw and ve.name == "PE":
                        continue
                    self._wait(hw, ve, ve.n - 1)
                else:
                    for q in range(max(0, ve.n - len(ve.sems)), ve.n):
                        self._wait(hw, ve, q)

    def final_wait(self, hwq, vengs):
        for ve in vengs:
            ns = len(ve.sems)
            if ve.n == 0:
                continue
            last = ve.n - 1
            for slot in range(ns):
                s = last - ((last - slot) % ns)
                if s >= 0:
                    hwq.h.wait_ge(ve.sems[slot], (s // ns + 1) * ve.inc)


class Rot:
    def __init__(self, items):
        self.items = items
        self.i = 0

    def next(self):
        it = self.items[self.i % len(self.items)]
        self.i += 1
        return it


class RotView:
    def __init__(self, rot, fn):
        self.rot = rot
        self.fn = fn

    def next(self):
        t, r = self.rot.next()
        return self.fn(t), r


def build_program(n_layers=2, stop_after=None, debug=False):
    nc = bass.Bass("TRN2", target_bir_lowering=False)

    def din(name, shape):
        return nc.dram_tensor(name, list(shape), F32, kind="ExternalInput").ap()

    def dout(name, shape):
        return nc.dram_tensor(name, list(shape), F32, kind="ExternalOutput").ap()

    def dscr(name, shape, dt):
        return nc.dram_tensor(name, list(shape), dt, kind=("ExternalOutput" if debug else "Internal")).ap()

    xp = din("xp", [NT, D])
    xs = din("xs", [SMP, D])
    cak = din("cak", [2, 512, D])
    cav = din("cav", [2, 512, D])
    cbk = din("cbk", [2, 4096, D])
    cbv = din("cbv", [2, 4096, D])
    cpl = din("cpl", [128, 8])
    csl = din("csl", [128, 8, 2])
    w_qkv = din("w_qkv", [2, D, 3 * D])
    w_o = din("w_o", [2, D, D])
    n1g = din("n1g", [2, D])
    n2g = din("n2g", [2, D])
    w_ada = din("w_ada", [2, D, 6 * D])
    b_ada = din("b_ada", [2, 6 * D])
    qng = din("qng", [1, 64])
    kng = din("kng", [1, 64])
    wrel = din("wrel", [16, 128, WRELW])
    wgd = din("wgd", [1, D, FF])
    wud = din("wud", [1, D, FF])
    wdd = din("wdd", [1, FF, D])
    wrt = din("wrt", [D, NE])
    wge = din("wge", [NE, D, FF])
    wue = din("wue", [NE, D, FF])
    wde = din("wde", [NE, FF, D])
    ident_d = din("ident", [128, 128])
    negtri_d = din("negtri", [128, 128])
    msb_d = din("msb", [128, 8, 512])
    msmp_d = din("msmp", [32, 32])
    pm_d = din("pm", [128, 2])

    yp = dout("yp", [NT // 2, D])
    ys = dout("ys", [SMP, D])
    akp = dout("akp", [512, D])
    avp = dout("avp", [512, D])
    aks = dout("aks", [2, 512, D])
    avs = dout("avs", [2, 512, D])
    bkp = dout("bkp", [NT, D])
    bvp = dout("bvp", [NT, D])
    bks = dout("bks", [SMP, D])
    bvs = dout("bvs", [SMP, D])

    QT = [dscr(f"QT{l}", [8, 128, NTOT], BF16) for l in range(2)]
    KT = [dscr(f"KT{l}", [8, 128, NTOT], BF16) for l in range(2)]
    VB = [dscr("VB0", [NTOT, D], BF16), None]
    VBm = dscr("VBm", [8, 128, NT // 128 + 1, 128], BF16)
    X1 = dscr("X1", [NTOT, D], F32)
    X2 = dscr("X2", [NTOT, D], F32)
    X3 = dscr("X3", [NT // 2 + SMP, D], F32)
    XN2T = [dscr("XN2T0", [8, 128, NTOT], BF16), dscr("XN2T1", [8, 128, NT // 2 + SMP], BF16)]
    CKT = dscr("CKT", [2, 8, 128, 4096], BF16)

    def rl(n):
        return [Res() for _ in range(n)]

    r_QT = [rl(17), rl(17)]
    r_KT = [rl(17), rl(17)]
    r_VB = [rl(17), rl(17)]
    r_X1 = rl(17)
    r_X2 = rl(17)
    r_X3 = rl(9)
    r_XN2T = [rl(17), rl(9)]
    r_CKT = rl(2)
    r_out = Res()

    with ExitStack() as top:
        S = Sched(nc, top)
        I = S.issue
        PE, ACT, DVE, POOL, DSP, DPOOL = S.PE, S.ACT, S.DVE, S.POOL, S.DSP, S.DPOOL

        uniq = [0]

        def sbt(es, name, shape, dt):
            uniq[0] += 1
            return es.enter_context(nc.sbuf_tensor(f"sb{uniq[0]}_{name}", list(shape), dt)), Res()

        def pst(es, name, shape, dt):
            uniq[0] += 1
            return es.enter_context(nc.psum_tensor(f"ps{uniq[0]}_{name}", list(shape), dt)), Res()

        identf, r_identf = sbt(top, "identf", [128, 128], F32)
        identb, r_identb = sbt(top, "identb", [128, 128], BF16)
        onesb, r_onesb = sbt(top, "onesb", [128, 128], BF16)
        negones, r_negones = sbt(top, "negones", [128, 128], BF16)
        negtri, r_negtri = sbt(top, "negtri", [128, 128], BF16)
        pm, r_pm = sbt(top, "pm", [128, 2], F32)
        I(DSP, lambda h: h.dma_start(out=identf[:], in_=ident_d), writes=[r_identf])
        I(DSP, lambda h: h.dma_start(out=pm[:], in_=pm_d), writes=[r_pm])
        I(DPOOL, lambda h: h.dma_start(out=identb[:], in_=ident_d), writes=[r_identb])
        I(DPOOL, lambda h: h.dma_start(out=negtri[:], in_=negtri_d), writes=[r_negtri])
        I(POOL, lambda h: h.memset(onesb[:], 1.0), writes=[r_onesb])
        I(POOL, lambda h: h.memset(negones[:], -1.0), writes=[r_negones])

        evac_flip = [0]
        dumped = set()

        def dump(name, ap, res, shape, dt=F32):
            if not debug or name in dumped:
                return
            dumped.add(name)
            d = nc.dram_tensor("dbg_" + name, list(shape), dt, kind="ExternalOutput").ap()
            I(DSP, lambda h: h.dma_start(out=d, in_=ap), reads=[res], writes=[Res()])

        def evac(out_ap, in_ap, reads, writes, eng=None):
            if eng is None:
                eng = ACT if (evac_flip[0] % 2 == 0) else DVE
                evac_flip[0] += 1
            if eng is ACT:
                I(ACT, lambda h: h.copy(out=out_ap, in_=in_ap), reads=reads, writes=writes)
            else:
                I(eng, lambda h: h.tensor_copy(out=out_ap, in_=in_ap), reads=reads, writes=writes)

        def tok_rows(t):
            if t < NTILE:
                return t * TS, TS, 4, 128
            return NT, SMP, 1, SMP

        for l in range(n_layers):
            with ExitStack() as lay:
                G2 = [sbt(lay, f"G2_{s}", [128, D], F32) for s in range(2)]
                gates, r_gates = sbt(lay, "gates", [128, 33, NE], F32)
                with ExitStack() as att:
                    AD = [[None] * 6 for _ in range(2)]
                    a1s = att.enter_context(ExitStack())
                    for s in range(2):
                        for v in (2, 3, 4):
                            AD[s][v] = sbt(att, f"AD{s}_{v}", [128, D], F32)
                        AD[s][5] = G2[s]
                    for s in range(2):
                        for v in (0, 1):
                            AD[s][v] = sbt(a1s, f"AD{s}_{v}", [128, D], F32)

                    with ExitStack() as es:
                        cp_t, r_cp = sbt(es, "cp_t", [128, 8], F32)
                        cs_t, r_cs = sbt(es, "cs_t", [128, 8, 2], F32)
                        LP, r_LP = sbt(es, "LP", [128, 8, 128], BF16)
                        LS, r_LS = sbt(es, "LS", [128, 8, 64], BF16)
                        bb, r_bb = sbt(es, "bb", [128, 6 * D], F32)
                        ng1, r_ng1 = sbt(es, "ng1", [128, D], F32)
                        ng2, r_ng2 = sbt(es, "ng2", [128, D], F32)
                        wch = Rot([sbt(es, f"wch{i}", [128, 8, 512], BF16) for i in range(2)])
                        pA = Rot([pst(es, f"pA{i}", [128, 512], F32) for i in range(4)])
                        I(DSP, lambda h: h.dma_start(out=cp_t[:], in_=cpl), writes=[r_cp])
                        I(DSP, lambda h: h.dma_start(out=cs_t[:], in_=csl), writes=[r_cs])
                        I(DSP, lambda h: h.dma_start(out=bb[:], in_=b_ada[l, :].partition_broadcast(128)), writes=[r_bb])
                        I(DSP, lambda h: h.dma_start(out=ng1[:], in_=n1g[l, :].partition_broadcast(128)), writes=[r_ng1])
                        I(DSP, lambda h: h.dma_start(out=ng2[:], in_=n2g[l, :].partition_broadcast(128)), writes=[r_ng2])
                        I(ACT, lambda h: h.activation(out=cp_t[:], in_=cp_t[:], func=AF.Silu), reads=[r_cp], writes=[r_cp])
                        I(ACT, lambda h: h.activation(out=cs_t[:], in_=cs_t[:], func=AF.Silu), reads=[r_cs], writes=[r_cs])
                        for k in range(8):
                            I(DVE, lambda h: h.tensor_scalar(out=LP[:, k, :], in0=onesb[:, :], scalar1=cp_t[:, k:k + 1],
                                                             scalar2=None, op0=ALU.mult),
                              reads=[r_onesb, r_cp], writes=[r_LP])
                            for s in range(2):
                                I(DVE, lambda h: h.tensor_scalar(out=LS[:, k, s * 32:(s + 1) * 32], in0=onesb[:, 0:32],
                                                                 scalar1=cs_t[:, k, s:s + 1], scalar2=None, op0=ALU.mult),
                                  reads=[r_onesb, r_cs], writes=[r_LS])
                        for c in range(12):
                            wt, r_wt = wch.next()
                            I(DPOOL, lambda h: h.dma_start(
                                out=wt[:], in_=w_ada[l, :, c * 512:(c + 1) * 512].rearrange("(k p) n -> p k n", p=128)),
                              writes=[r_wt])
                            vec, half = c // 2, c % 2
                            cols = slice(half * 512, (half + 1) * 512)
                            for s, (L_, r_L, P_) in enumerate(((LP, r_LP, 128), (LS, r_LS, 64))):
                                ps, r_ps = pA.next()
                                for k in range(8):
                                    I(PE, lambda h: h.matmul(ps[:P_, :], lhsT=L_[:, k, :P_], rhs=wt[:, k, :],
                                                             start=(k == 0), stop=(k == 7)),
                                      reads=[r_L, r_wt], writes=[r_ps])
                                dst, r_dst = AD[s][vec]
                                I(DVE, lambda h: h.tensor_tensor(out=dst[:P_, cols], in0=ps[:P_, :],
                                                                 in1=bb[:P_, c * 512:(c + 1) * 512], op=ALU.add),
                                  reads=[r_ps, r_bb], writes=[r_dst])
                                if vec in (1, 4):
                                    ng, r_ng = (ng1, r_ng1) if vec == 1 else (ng2, r_ng2)
                                    I(DVE, lambda h: h.scalar_tensor_tensor(out=dst[:P_, cols], in0=dst[:P_, cols], scalar=1.0,
                                                                            in1=ng[:P_, cols], op0=ALU.add, op1=ALU.mult),
                                      reads=[r_dst, r_ng], writes=[r_dst])

                    S.barrier()
                    for v in range(6):
                        dump(f"AD0_{v}_l{l}", AD[0][v][0][:, :], AD[0][v][1], [128, D])
                        dump(f"AD1_{v}_l{l}", AD[1][v][0][:64, :], AD[1][v][1], [64, D])
                    def norm_to_T(es_bufs, x_ap, r_x, GM, SH, P_, defer_T=None):
                        (junk, r_junk), ssr, tmpr, xnr, pTr, xnTr = es_bufs
                        ss, r_ss = ssr.next()
                        tmpf, r_tmpf = tmpr.next()
                        xn, r_xn = xnr.next()
                        pT, r_pT = pTr.next()
                        xnT, r_xnT = xnTr.next()
                        I(ACT, lambda h: h.activation(out=junk[:P_, :], in_=x_ap, func=AF.Square, accum_out=ss[:P_, 0:1]),
                          reads=[r_x], writes=[r_junk, r_ss])
                        I(ACT, lambda h: h.activation(out=ss[:P_, 1:2], in_=ss[:P_, 0:1], func=AF.Ln, scale=1.0 / D, bias=EPS),
                          reads=[r_ss], writes=[r_ss])
                        I(ACT, lambda h: h.activation(out=ss[:P_, 2:3], in_=ss[:P_, 1:2], func=AF.Exp, scale=-0.5),
                          reads=[r_ss], writes=[r_ss])
                        I(DVE, lambda h: h.scalar_tensor_tensor(out=tmpf[:P_, :], in0=x_ap, scalar=ss[:P_, 2:3],
                                                                in1=GM[0][:P_, :], op0=ALU.mult, op1=ALU.mult),
                          reads=[r_x, r_ss, GM[1]], writes=[r_tmpf])
                        I(POOL, lambda h: h.tensor_tensor(out=xn[:P_, :], in0=tmpf[:P_, :], in1=SH[0][:P_, :], op=ALU.add),
                          reads=[r_tmpf, SH[1]], writes=[r_xn])
                        def tpart():
                            for k in range(8):
                                I(PE, lambda h: h.transpose(out=pT[:, k, :P_], in_=xn[:P_, k * 128:(k + 1) * 128],
                                                            identity=identb[:P_, :P_]),
                                  reads=[r_xn, r_identb], writes=[r_pT])
                            evac(xnT[:, :, :P_], pT[:, :, :P_], [r_pT], [r_xnT])
                        if defer_T is not None:
                            defer_T.append(tpart)
                            return xnT, r_xnT
                        tpart()
                        dump(f"ss_l{l}", ss[:, :], r_ss, [128, 4])
                        dump(f"tmpf_l{l}", tmpf[:, :], r_tmpf, [128, D])
                        dump(f"xn_l{l}", xn[:, :], r_xn, [128, D], BF16)
                        dump(f"xnT_l{l}", xnT[:, :, :], r_xnT, [128, 8, 128], BF16)
                        return xnT, r_xnT

                    def norm_bufs(es, pfx, npT=1):
                        return (sbt(es, pfx + "junk", [128, D], F32),
                                Rot([sbt(es, f"{pfx}ss{i}", [128, 4], F32) for i in range(2)]),
                                Rot([sbt(es, f"{pfx}tmpf{i}", [128, D], F32) for i in range(1)]),
                                Rot([sbt(es, f"{pfx}xn{i}", [128, D], BF16) for i in range(2)]),
                                Rot([pst(es, f"{pfx}pT{i}", [128, 8, 128], BF16) for i in range(npT)]),
                                Rot([sbt(es, f"{pfx}xnT{i}", [128, 8, 128], BF16) for i in range(2)]))

                    x_src, r_xsrc = ((xp, xs), None) if l == 0 else (None, r_X2)

                    def x_rows(row0, P_, t):
                        if l == 0:
                            if t < NTILE:
                                return xp[row0:row0 + P_, :], []
                            return xs[row0 - NT:row0 - NT + P_, :], []
                        return X2[row0:row0 + P_, :], [r_X2[t]]

                    with ExitStack() as es:
                        wq, r_wq = sbt(es, "wq", [128, 8, 3 * D], BF16)
                        for c in range(6):
                            I(DPOOL, lambda h: h.dma_start(
                                out=wq[:, :, c * 512:(c + 1) * 512],
                                in_=w_qkv[l, :, c * 512:(c + 1) * 512].rearrange("(k p) n -> p k n", p=128)),
                              writes=[r_wq])
                        nb = norm_bufs(es, "a1", 2)
                        xbuf = Rot([sbt(es, f"a1x{i}", [128, D], F32) for i in range(3)])
                        qkvf = Rot([sbt(es, f"qkvf{i}", [128, 3 * D], F32) for i in range(2)])
                        qkb = Rot([sbt(es, f"qkb{i}", [128, 2 * D], BF16) for i in range(2)])
                        vbb = Rot([sbt(es, f"vbb{i}", [128, D], BF16) for i in range(2)])
                        qkT = Rot([sbt(es, f"qkT{i}", [128, 16, 128], BF16) for i in range(2)])
                        pQ = Rot([pst(es, f"pQ{i}", [128, 512], F32) for i in range(3)])
                        pQT = Rot([pst(es, f"pQT{i}", [128, 8, 128], BF16) for i in range(2)])
                        if l == 0:
                            gq, r_gq = sbt(es, "gq", [128, 64], F32)
                            gk, r_gk = sbt(es, "gk", [128, 64], F32)
                            sqb, r_sqb = sbt(es, "sqb", [128, 2 * D], F32)
                            ssq, r_ssq = sbt(es, "ssq", [128, 32], F32)
                            I(DSP, lambda h: h.dma_start(out=gq[:], in_=qng[0, :].partition_broadcast(128)), writes=[r_gq])
                            I(DSP, lambda h: h.dma_start(out=gk[:], in_=kng[0, :].partition_broadcast(128)), writes=[r_gk])
                            I(DVE, lambda h: h.tensor_scalar(out=gq[:], in0=gq[:], scalar1=0.125, scalar2=None, op0=ALU.mult),
                              reads=[r_gq], writes=[r_gq])
                        subs_ = []
                        for t in range(17):
                            row0, ntok, nsub, P_ = tok_rows(t)
                            for sub in range(nsub):
                                subs_.append((t, sub, row0, P_))
                        stA1 = [dict() for _ in subs_]

                        deferred = []
                        TL_ = []

                        def DI(*a, **k):
                            deferred.append((a, k))

                        def flush_():
                            for a, k in deferred:
                                I(*a, **k)
                            deferred.clear()

                        def L_(n_):
                            t, sub, row0, P_ = subs_[n_]
                            r0 = row0 + sub * 128
                            xb, r_xb = xbuf.next()
                            src, rsrc = x_rows(r0, P_, t)
                            I(DSP, lambda h: h.dma_start(out=xb[:P_, :], in_=src), reads=rsrc, writes=[r_xb])
                            stA1[n_]['xb'] = (xb, r_xb)

                        def F_(n_):
                            t, sub, row0, P_ = subs_[n_]
                            r0 = row0 + sub * 128
                            stt_ = stA1[n_]
                            xb, r_xb = stt_['xb']
                            xnT, r_xnT = norm_to_T(nb, xb[:P_, :], r_xb, AD[1 if t == 16 else 0][1], AD[1 if t == 16 else 0][0], P_,
                                                   defer_T=TL_)
                            stt_['xnT'] = (xnT, r_xnT)

                        def G1_(n_):
                            t, sub, row0, P_ = subs_[n_]
                            r0 = row0 + sub * 128
                            stt_ = stA1[n_]
                            xnT, r_xnT = stt_['xnT']
                            qf, r_qf = qkvf.next()
                            for cc in range(6):
                                ps, r_ps = pQ.next()
                                for k in range(8):
                                    I(PE, lambda h: h.matmul(ps[:P_, :], lhsT=xnT[:, k, :P_], rhs=wq[:, k, cc * 512:(cc + 1) * 512],
                                                             start=(k == 0), stop=(k == 7)),
                                      reads=[r_xnT, r_wq], writes=[r_ps])
                                evac(qf[:P_, cc * 512:(cc + 1) * 512], ps[:P_, :], [r_ps], [r_qf])
                            stt_['qf'] = (qf, r_qf)

                        def G2a_(n_):
                            t, sub, row0, P_ = subs_[n_]
                            r0 = row0 + sub * 128
                            stt_ = stA1[n_]
                            qf, r_qf = stt_['qf']
                            qb, r_qb = qkb.next()
                            vb, r_vb = vbb.next()
                            if l == 0:
                                I(ACT, lambda h: h.activation(out=sqb[:P_, :], in_=qf[:P_, 0:2 * D], func=AF.Square),
                                  reads=[r_qf], writes=[r_sqb])
                                I(DVE, lambda h: h.tensor_reduce(out=ssq[:P_, :], in_=sqb[:P_, :].rearrange("p (h d) -> p h d", d=64),
                                                                 axis=AX.X, op=ALU.add),
                                  reads=[r_sqb], writes=[r_ssq])
                                I(ACT, lambda h: h.activation(out=ssq[:P_, :], in_=ssq[:P_, :], func=AF.Ln, scale=1.0 / 64, bias=EPS),
                                  reads=[r_ssq], writes=[r_ssq])
                                I(ACT, lambda h: h.activation(out=ssq[:P_, :], in_=ssq[:P_, :], func=AF.Exp, scale=-0.5),
                                  reads=[r_ssq], writes=[r_ssq])
                                for qi, (g_, r_g) in enumerate(((gq, r_gq), (gk, r_gk))):
                                    fv = qf[:P_, qi * D:(qi + 1) * D].rearrange("p (h d) -> p h d", d=64)
                                    I(DVE, lambda h: h.tensor_tensor(
                                        out=fv, in0=fv,
                                        in1=ssq[:P_, qi * 16:(qi + 1) * 16].unsqueeze(2).to_broadcast([P_, 16, 64]), op=ALU.mult),
                                      reads=[r_qf, r_ssq], writes=[r_qf])
                                    I(DVE, lambda h: h.tensor_tensor(
                                        out=fv, in0=fv, in1=g_[:P_, :].unsqueeze(1).to_broadcast([P_, 16, 64]), op=ALU.mult),
                                      reads=[r_qf, r_g], writes=[r_qf])
                                I(ACT, lambda h: h.copy(out=qb[:P_, :], in_=qf[:P_, 0:2 * D]), reads=[r_qf], writes=[r_qb])
                            else:
                                I(ACT, lambda h: h.activation(out=qb[:P_, 0:D], in_=qf[:P_, 0:D], func=AF.Copy, scale=0.125),
                                  reads=[r_qf], writes=[r_qb])
                                I(POOL, lambda h: h.tensor_copy(out=qb[:P_, D:2 * D], in_=qf[:P_, D:2 * D]), reads=[r_qf], writes=[r_qb])
                            I(POOL if l == 0 else DVE, lambda h: h.tensor_copy(out=vb[:P_, :], in_=qf[:P_, 2 * D:3 * D]), reads=[r_qf], writes=[r_vb])
                            if l == 0:
                                if t == NTILE - 1:
                                    DI(DSP, lambda h: h.dma_start(out=akp[sub * 128:(sub + 1) * 128, :], in_=qf[:P_, D:2 * D]),
                                      reads=[r_qf], writes=[r_out])
                                    DI(DSP, lambda h: h.dma_start(out=avp[sub * 128:(sub + 1) * 128, :], in_=qf[:P_, 2 * D:3 * D]),
                                      reads=[r_qf], writes=[r_out])
                                if t == 16:
                                    for s in range(2):
                                        DI(DSP, lambda h, s=s: h.dma_start(out=aks[s, 480:512, :], in_=qf[s * 32:(s + 1) * 32, D:2 * D]),
                                          reads=[r_qf], writes=[r_out])
                                        DI(DSP, lambda h, s=s: h.dma_start(out=avs[s, 480:512, :], in_=qf[s * 32:(s + 1) * 32, 2 * D:3 * D]),
                                          reads=[r_qf], writes=[r_out])
                                        DI(DSP, lambda h, s=s: h.dma_start(out=aks[s, 0:480, :], in_=cak[s, 32:512, :]), writes=[r_out])
                                        DI(DSP, lambda h, s=s: h.dma_start(out=avs[s, 0:480, :], in_=cav[s, 32:512, :]), writes=[r_out])
                            else:
                                if t < NTILE:
                                    DI(DSP, lambda h: h.dma_start(out=bkp[r0:r0 + 128, :], in_=qf[:P_, D:2 * D]), reads=[r_qf], writes=[r_out])
                                    DI(DSP, lambda h: h.dma_start(out=bvp[r0:r0 + 128, :], in_=qf[:P_, 2 * D:3 * D]), reads=[r_qf], writes=[r_out])
                                else:
                                    DI(DSP, lambda h: h.dma_start(out=bks[:, :], in_=qf[:P_, D:2 * D]), reads=[r_qf], writes=[r_out])
                                    DI(DSP, lambda h: h.dma_start(out=bvs[:, :], in_=qf[:P_, 2 * D:3 * D]), reads=[r_qf], writes=[r_out])
                            if l == 0:
                                DI(DSP, lambda h: h.dma_start(out=VB[0][r0:r0 + P_, :], in_=vb[:P_, :]), reads=[r_vb], writes=[r_VB[0][t]])
                            else:
                                DI(DSP, lambda h: h.dma_start(out=VBm[:, 0:P_, r0 // 128, :].rearrange("m p f -> p m f"),
                                                             in_=vb[:P_, :].rearrange("p (m f) -> p m f", f=128)),
                                  reads=[r_vb], writes=[r_VB[1][t]])
                            stt_['qb'] = (qb, r_qb)

                        def G2b_(n_):
                            t, sub, row0, P_ = subs_[n_]
                            r0 = row0 + sub * 128
                            stt_ = stA1[n_]
                            qb, r_qb = stt_['qb']
                            qt, r_qt = qkT.next()
                            for hf in range(2):
                                pt, r_pt = pQT.next()
                                for m in range(8):
                                    I(PE, lambda h: h.transpose(out=pt[:, m, :P_], in_=qb[:P_, hf * D + m * 128: hf * D + (m + 1) * 128],
                                                                identity=identb[:P_, :P_]),
                                      reads=[r_qb, r_identb], writes=[r_pt])
                                evac(qt[:, hf * 8:(hf + 1) * 8, :P_], pt[:, :, :P_], [r_pt], [r_qt])
                            DI(DSP, lambda h: h.dma_start(out=QT[l][:, :, r0:r0 + P_].rearrange("m p t -> p m t"), in_=qt[:, 0:8, :P_]),
                              reads=[r_qt], writes=[r_QT[l][t]])
                            DI(DSP, lambda h: h.dma_start(out=KT[l][:, :, r0:r0 + P_].rearrange("m p t -> p m t"), in_=qt[:, 8:16, :P_]),
                              reads=[r_qt], writes=[r_KT[l][t]])

                        N_ = len(subs_)
                        for step_ in range(N_ + 4):
                            flush_()
                            if 0 <= step_ - 3 < N_:
                                G2a_(step_ - 3)
                            if 0 <= step_ - 2 < N_:
                                G1_(step_ - 2)
                            if 0 <= step_ - 1 < N_:
                                F_(step_ - 1)
                            if step_ < N_:
                                L_(step_)
                            if 0 <= step_ - 3 < N_:
                                G2b_(step_ - 3)
                            for f_ in TL_:
                                f_()
                            TL_.clear()
                        flush_()
                    S.barrier()
                    a1s.close()
                    if stop_after == ("A1", l):
                        break

                    with ExitStack() as es:
                        wo, r_wo = sbt(es, "wo", [128, 8, D], BF16)
                        I(DPOOL, lambda h: h.dma_start(out=wo[:], in_=w_o[l].rearrange("(k p) n -> p k n", p=128)), writes=[r_wo])
                        nb = norm_bufs(es, "a2", 1)
                        xbuf = Rot([sbt(es, f"a2x{i}", [128, D], F32) for i in range(4)])
                        x1b = Rot([sbt(es, f"a2x1{i}", [128, D], F32) for i in range(2)])
                        OTs, r_OTs = sbt(es, "OTs", [128, 8, TS], BF16)
                        pY = Rot([pst(es, f"pY{i}", [128, 512], F32) for i in range(3 if l == 0 else 4)])
                        if l == 1:
                            wrf, r_wrf = sbt(es, "wrf", [128, 8, NE], F32)
                            I(DSP, lambda h: h.dma_start(out=wrf[:], in_=wrt.rearrange("(k p) e -> p k e", p=128)), writes=[r_wrf])
                            xn2f, r_xn2f = sbt(es, "xn2f", [128, D], F32)
                            xn2fT, r_xn2fT = sbt(es, "xn2fT", [128, 8, 128], F32)
                            rt, r_rt = sbt(es, "rt", [128, 64], F32)

                        def post_attn(t_out, nsub, P_, x_loader, set_i, sub_base):
                            stp = [dict() for _ in range(nsub)]

                            pdef = []

                            def PDI(*a, **k):
                                pdef.append((a, k))

                            def pflush():
                                for a, k in pdef:
                                    I(*a, **k)
                                pdef.clear()

                            def P0_(sub):
                                stp[sub]['xb'] = x_loader(sub)

                            def P1_(sub):
                                xb, r_xb = stp[sub]['xb']
                                x1, r_x1 = x1b.next()
                                for half in range(2):
                                    py, r_py = pY.next()
                                    for m in range(8):
                                        I(PE, lambda h: h.matmul(py[:P_, :], lhsT=OTs[:, m, sub * 128: sub * 128 + P_],
                                                                 rhs=wo[:, m, half * 512:(half + 1) * 512], start=(m == 0), stop=(m == 7)),
                                          reads=[r_OTs, r_wo], writes=[r_py])
                                    cs_ = slice(half * 512, (half + 1) * 512)
                                    I(DVE, lambda h: h.tensor_tensor(out=x1[:P_, cs_], in0=py[:P_, :], in1=AD[set_i][2][0][:P_, cs_], op=ALU.mult),
                                      reads=[r_py, AD[set_i][2][1]], writes=[r_x1])
                                I(POOL, lambda h: h.tensor_tensor(out=x1[:P_, :], in0=x1[:P_, :], in1=xb[:P_, :], op=ALU.add),
                                  reads=[r_x1, r_xb], writes=[r_x1])
                                if l == 0:
                                    rr = t_out * TS + sub * 128 if t_out < NTILE else NT
                                    PDI(DSP, lambda h: h.dma_start(out=X1[rr:rr + P_, :], in_=x1[:P_, :]), reads=[r_x1], writes=[r_X1[t_out]])
                                else:
                                    rr = t_out * TS + sub * 128 if t_out < 8 else NT // 2
                                    PDI(DSP, lambda h: h.dma_start(out=X3[rr:rr + P_, :], in_=x1[:P_, :]), reads=[r_x1], writes=[r_X3[t_out]])
                                stp[sub].update(x1=(x1, r_x1), rr=rr)

                            def P2_(sub):
                                x1, r_x1 = stp[sub]['x1']
                                rr = stp[sub]['rr']
                                xnT, r_xnT = norm_to_T(nb, x1[:P_, :], r_x1, AD[set_i][4], AD[set_i][3], P_)
                                PDI(DSP, lambda h: h.dma_start(out=XN2T[l][:, :, rr:rr + P_].rearrange("m p t -> p m t"), in_=xnT[:, :, :P_]),
                                  reads=[r_xnT], writes=[r_XN2T[l][t_out]])
                                if l == 1:
                                    sg = sub_base + sub
                                    ssl = nb[1].items[(nb[1].i - 1) % 2]
                                    I(DVE, lambda h: h.scalar_tensor_tensor(out=xn2f[:P_, :], in0=x1[:P_, :], scalar=ssl[0][:P_, 2:3],
                                                                            in1=AD[set_i][4][0][:P_, :], op0=ALU.mult, op1=ALU.mult),
                                      reads=[r_x1, ssl[1], AD[set_i][4][1]], writes=[r_xn2f])
                                    I(POOL, lambda h: h.tensor_tensor(out=xn2f[:P_, :], in0=xn2f[:P_, :], in1=AD[set_i][3][0][:P_, :], op=ALU.add),
                                      reads=[r_xn2f, AD[set_i][3][1]], writes=[r_xn2f])
                                    for hf in range(2):
                                        py, r_py = pY.next()
                                        for k in range(4):
                                            kk = hf * 4 + k
                                            I(PE, lambda h: h.transpose(out=py[:, k * 128:k * 128 + P_], in_=xn2f[:P_, kk * 128:(kk + 1) * 128],
                                                                        identity=identf[:P_, :P_]),
                                              reads=[r_xn2f, r_identf], writes=[r_py])
                                        evac(xn2fT[:, hf * 4:(hf + 1) * 4, :P_], py[:, :].rearrange("p (k t) -> p k t", t=128)[:, :, :P_],
                                             [r_py], [r_xn2fT])
                                    py, r_py = pY.next()
                                    for k in range(8):
                                        I(PE, lambda h: h.matmul(py[:P_, 0:NE], lhsT=xn2fT[:, k, :P_], rhs=wrf[:, k, :],
                                                                 start=(k == 0), stop=(k == 7)),
                                          reads=[r_xn2fT, r_wrf], writes=[r_py])
                                    I(DVE, lambda h: h.tensor_copy(out=rt[:P_, 0:8], in_=py[:P_, 0:NE]), reads=[r_py], writes=[r_rt])
                                    I(DVE, lambda h: h.tensor_reduce(out=rt[:P_, 32:33], in_=rt[:P_, 0:8], axis=AX.X, op=ALU.max),
                                      reads=[r_rt], writes=[r_rt])
                                    I(DVE, lambda h: h.tensor_scalar(out=rt[:P_, 8:16], in0=rt[:P_, 0:8], scalar1=rt[:P_, 32:33], scalar2=None,
                                                                     op0=ALU.is_equal), reads=[r_rt], writes=[r_rt])
                                    I(DVE, lambda h: h.scalar_tensor_tensor(out=rt[:P_, 16:24], in0=rt[:P_, 8:16], scalar=-1e30, in1=rt[:P_, 0:8],
                                                                            op0=ALU.mult, op1=ALU.add), reads=[r_rt], writes=[r_rt])
                                    I(DVE, lambda h: h.tensor_reduce(out=rt[:P_, 33:34], in_=rt[:P_, 16:24], axis=AX.X, op=ALU.max),
                                      reads=[r_rt], writes=[r_rt])
                                    I(DVE, lambda h: h.tensor_scalar(out=rt[:P_, 24:32], in0=rt[:P_, 16:24], scalar1=rt[:P_, 33:34], scalar2=None,
                                                                     op0=ALU.is_equal), reads=[r_rt], writes=[r_rt])
                                    I(DVE, lambda h: h.tensor_tensor(out=rt[:P_, 34:35], in0=rt[:P_, 32:33], in1=rt[:P_, 33:34], op=ALU.subtract),
                                      reads=[r_rt], writes=[r_rt])
                                    I(ACT, lambda h: h.activation(out=rt[:P_, 34:35], in_=rt[:P_, 34:35], func=AF.Exp), reads=[r_rt], writes=[r_rt])
                                    I(DVE, lambda h: h.tensor_scalar(out=rt[:P_, 34:35], in0=rt[:P_, 34:35], scalar1=1.0, scalar2=None, op0=ALU.add),
                                      reads=[r_rt], writes=[r_rt])
                                    I(DVE, lambda h: h.reciprocal(out=rt[:P_, 35:36], in_=rt[:P_, 34:35]), reads=[r_rt], writes=[r_rt])
                                    I(DVE, lambda h: h.tensor_scalar(out=rt[:P_, 34:35], in0=rt[:P_, 35:36], scalar1=-1.0, scalar2=1.0,
                                                                     op0=ALU.mult, op1=ALU.add), reads=[r_rt], writes=[r_rt])
                                    I(DVE, lambda h: h.tensor_scalar(out=rt[:P_, 8:16], in0=rt[:P_, 8:16], scalar1=rt[:P_, 34:35], scalar2=None,
                                                                     op0=ALU.mult), reads=[r_rt], writes=[r_rt])
                                    I(DVE, lambda h: h.scalar_tensor_tensor(out=gates[:P_, sg, :], in0=rt[:P_, 24:32], scalar=rt[:P_, 35:36],
                                                                            in1=rt[:P_, 8:16], op0=ALU.mult, op1=ALU.add),
                                      reads=[r_rt], writes=[r_gates])

                            la_ = 2 if l == 0 else 1
                            for q_ in range(min(la_, nsub)):
                                P0_(q_)
                            for q_ in range(nsub + 1):
                                pflush()
                                if q_ + la_ < nsub:
                                    P0_(q_ + la_)
                                if q_ < nsub:
                                    P1_(q_)
                                if q_ - 1 >= 0:
                                    P2_(q_ - 1)
                            pflush()

                        if l == 0:
                            with ExitStack() as e2:
                                wrl, r_wrl = sbt(e2, "wrl", [128, 16, WRELW], BF16)
                                for hh in range(0, 16, 4):
                                    I(DPOOL, lambda h: h.dma_start(out=wrl[:, hh:hh + 4, :], in_=wrel[hh:hh + 4].rearrange("h p w -> p h w")),
                                      writes=[r_wrl])
                                PTb = Rot([sbt(e2, f"PTb{i}", [128, TS], BF16) for i in range(3)])
                                rec, r_rec = sbt(e2, "rec", [128, TS], F32)
                                e3 = e2.enter_context(ExitStack())
                                qTb = Rot([sbt(e3, f"qTb{i}", [128, 8, TS], BF16) for i in range(1)])
                                KTr = [sbt(e3, f"KTr{i}", [128, 8, TS], BF16) for i in range(2)]
                                Vr = [sbt(e3, f"Vr{i}", [128, 4, D], BF16) for i in range(2)]
                                pS = pY
                                pO = Rot([pst(e2, f"pO{i}", [128, 512], F32) for i in range(2)])
                                pR = Rot([pst(e2, f"pR{i}", [128, 512], F32) for i in range(2)])

                                def band_run(entries):
                                    flat = []
                                    for (hd, q_ap_fn, units, ncols, out_cols) in entries:
                                        nu = len(units)
                                        for i, u in enumerate(units):
                                            flat.append((hd, q_ap_fn, ncols, out_cols, i == 0, i == nu - 1, u))
                                    st = {}

                                    def A(k):
                                        hd, q_ap_fn, ncols, out_cols, first, last, (kT_fn, v_ap, nk, woff, lo, hi, ms, rds) = flat[k]
                                        r0 = (hd % 2) * 64
                                        ps, r_ps = pS.next()
                                        q_ap, r_q = q_ap_fn(r0, lo, hi)
                                        I(PE, lambda h: h.matmul(ps[:nk, lo:hi], lhsT=kT_fn(r0), rhs=q_ap, start=True, stop=False),
                                          reads=rds + [r_q], writes=[r_ps])
                                        I(PE, lambda h: h.matmul(ps[:nk, lo:hi], lhsT=identb[:nk, :nk], rhs=wrl[:nk, hd, woff + lo:woff + hi],
                                                                 start=False, stop=True),
                                          reads=[r_identb, r_wrl], writes=[r_ps])
                                        st[k] = (ps, r_ps)

                                    def B(k):
                                        hd, q_ap_fn, ncols, out_cols, first, last, (kT_fn, v_ap, nk, woff, lo, hi, ms, rds) = flat[k]
                                        ps, r_ps = st[k]
                                        pt, r_pt = PTb.next()
                                        I(ACT, lambda h: h.activation(out=pt[:nk, lo:hi], in_=ps[:nk, lo:hi], func=AF.Exp), reads=[r_ps], writes=[r_pt])
                                        if ms is not None:
                                            rows, c0 = ms
                                            I(POOL, lambda h: h.memset(pt[rows:rows + 64, c0:c0 + 64], 0.0), writes=[r_pt])
                                        st[k] = (pt, r_pt)

                                    def C(k):
                                        hd, q_ap_fn, ncols, out_cols, first, last, (kT_fn, v_ap, nk, woff, lo, hi, ms, rds) = flat[k]
                                        m, r0 = hd // 2, (hd % 2) * 64
                                        pt, r_pt = st.pop(k)
                                        if first:
                                            st["po"] = pO.next()
                                            st["pr"] = pR.next()
                                        po, r_po = st["po"]
                                        pr, r_pr = st["pr"]
                                        I(PE, lambda h: h.matmul(po[:, lo:hi], lhsT=v_ap, rhs=pt[:nk, lo:hi], start=first, stop=last),
                                          reads=rds + [r_pt], writes=[r_po])
                                        I(PE, lambda h: h.matmul(pr[:, lo:hi], lhsT=onesb[:nk, :], rhs=pt[:nk, lo:hi], start=first, stop=last),
                                          reads=[r_onesb, r_pt], writes=[r_pr])
                                        if last:
                                            I(DVE, lambda h: h.reciprocal(out=rec[r0:r0 + 64, :ncols], in_=pr[r0:r0 + 64, :ncols]),
                                              reads=[r_pr], writes=[r_rec])
                                            I(DVE, lambda h: h.tensor_tensor(out=OTs[r0:r0 + 64, m, out_cols], in0=po[r0:r0 + 64, :ncols],
                                                                             in1=rec[r0:r0 + 64, :ncols], op=ALU.mult),
                                              reads=[r_po, r_rec], writes=[r_OTs])

                                    n = len(flat)
                                    A(0)
                                    if n > 1:
                                        A(1)
                                    for k in range(n):
                                        if k + 2 < n:
                                            A(k + 2)
                                        B(k)
                                        C(k)

                                COLS = [(0, 128), (0, 256), (0, 384), (0, 512), (0, 512), (128, 512), (256, 512), (384, 512)]
                                for t in range(NTILE):
                                    qT, r_qT = qTb.next()
                                    I(DSP, lambda h: h.dma_start(out=qT[:], in_=QT[0][:, :, t * TS:(t + 1) * TS].rearrange("m p t -> p m t")),
                                      reads=[r_QT[0][t]], writes=[r_qT])
                                    kc, r_kc = KTr[t % 2]
                                    vc, r_vc = Vr[t % 2]
                                    I(DSP, lambda h: h.dma_start(out=kc[:], in_=KT[0][:, :, t * TS:(t + 1) * TS].rearrange("m p t -> p m t")),
                                      reads=[r_KT[0][t]], writes=[r_kc])
                                    I(DSP, lambda h: h.dma_start(out=vc[:], in_=VB[0][t * TS:(t + 1) * TS, :].rearrange("(k p) f -> p k f", p=128)),
                                      reads=[r_VB[0][t]], writes=[r_vc])
                                    entries = []
                                    for hd in range(16):
                                        m = hd // 2
                                        units = []
                                        order = [4, 5, 6, 7] + ([0, 1, 2, 3] if t > 0 else [])
                                        for kr in order:
                                            tt = t if kr >= 4 else t - 1
                                            ktl = kr % 4
                                            kb, r_kb = KTr[tt % 2]
                                            vb_, r_vb_ = Vr[tt % 2]
                                            lo, hi = COLS[kr]
                                            ms = (0, (2 * kr + 1) * 64) if kr <= 3 else (64, (2 * kr - 8) * 64)
                                            units.append((
                                                (lambda r0, kb=kb, ktl=ktl, m=m: kb[r0:r0 + 64, m, ktl * 128:(ktl + 1) * 128]),
                                                vb_[:, ktl, m * 128:(m + 1) * 128], 128, 639 - 128 * kr, lo, hi, ms, [r_kb, r_vb_]))
                                        entries.append((hd, (lambda r0, lo, hi, qT=qT, m=m, r_qT=r_qT: (qT[r0:r0 + 64, m, lo:hi], r_qT)),
                                                        units, TS, slice(0, TS)))
                                    band_run(entries)

                                    def xload(sub, t=t):
                                        xb, r_xb = xbuf.next()
                                        I(DSP, lambda h: h.dma_start(out=xb[:, :], in_=xp[t * TS + sub * 128: t * TS + (sub + 1) * 128, :]), writes=[r_xb])
                                        return xb, r_xb
                                    post_attn(t, 4, 128, xload, 0, 0)

                                S.barrier()
                                e3.close()
                                ckb, r_ckb = sbt(e2, "ckb", [128, D], BF16)
                                cKT, r_cKT = sbt(e2, "cKT", [128, 8, 512], BF16)
                                cV, r_cV = sbt(e2, "cV", [128, 4, D], BF16)
                                nKT, r_nKT = sbt(e2, "nKT", [128, 8, 32], BF16)
                                nV, r_nV = sbt(e2, "nV", [32, D], BF16)
                                qTs, r_qTs = sbt(e2, "qTs", [128, 8, 32], BF16)
                                pT2 = nb[4]
                                for s in range(2):
                                    c0 = NT + s * 32
                                    for kt in range(4):
                                        I(DPOOL, lambda h: h.dma_start(out=ckb[:], in_=cak[s, kt * 128:(kt + 1) * 128, :]), writes=[r_ckb])
                                        pt, r_pt = pT2.next()
                                        for m in range(8):
                                            I(PE, lambda h: h.transpose(out=pt[:, m, :], in_=ckb[:, m * 128:(m + 1) * 128], identity=identb[:]),
                                              reads=[r_ckb, r_identb], writes=[r_pt])
                                        evac(cKT[:, :, kt * 128:(kt + 1) * 128], pt[:], [r_pt], [r_cKT])
                                    I(DPOOL, lambda h: h.dma_start(out=cV[:], in_=cav[s].rearrange("(k p) f -> p k f", p=128)), writes=[r_cV])
                                    I(DSP, lambda h: h.dma_start(out=nKT[:], in_=KT[0][:, :, c0:c0 + 32].rearrange("m p t -> p m t")),
                                      reads=[r_KT[0][16]], writes=[r_nKT])
                                    I(DSP, lambda h: h.dma_start(out=nV[:], in_=VB[0][c0:c0 + 32, :]), reads=[r_VB[0][16]], writes=[r_nV])
                                    I(DSP, lambda h: h.dma_start(out=qTs[:], in_=QT[0][:, :, c0:c0 + 32].rearrange("m p t -> p m t")),
                                      reads=[r_QT[0][16]], writes=[r_qTs])
                                    entries = []
                                    for hd in range(16):
                                        m = hd // 2
                                        units = []
                                        for kt in range(4):
                                            units.append(((lambda r0, kt=kt, m=m: cKT[r0:r0 + 64, m, kt * 128:(kt + 1) * 128]),
                                                          cV[:, kt, m * 128:(m + 1) * 128], 128, 639 - 128 * kt, 0, 32, None, [r_cKT, r_cV]))
                                        units.append(((lambda r0, m=m: nKT[r0:r0 + 64, m, :]), nV[:, m * 128:(m + 1) * 128], 32, 127, 0, 32, None,
                                                      [r_nKT, r_nV]))
                                        entries.append((hd, (lambda r0, lo, hi, m=m: (qTs[r0:r0 + 64, m, lo:hi], r_qTs)), units, 32,
                                                        slice(s * 32, (s + 1) * 32)))
                                    band_run(entries)

                                def xload_s(sub):
                                    xb, r_xb = xbuf.next()
                                    I(DSP, lambda h: h.dma_start(out=xb[:SMP, :], in_=xs[:, :]), writes=[r_xb])
                                    return xb, r_xb
                                post_attn(16, 1, SMP, xload_s, 1, 0)
                                S.barrier()
                        else:
                            with ExitStack() as e2:
                                msb, r_msb = sbt(e2, "msb", [128, 8, 512], BF16)
                                msmp, r_msmp = sbt(e2, "msmp", [32, 32], BF16)
                                I(DPOOL, lambda h: h.dma_start(out=msb[:], in_=msb_d), writes=[r_msb])
                                I(DPOOL, lambda h: h.dma_start(out=msmp[:], in_=msmp_d), writes=[r_msmp])
                                qTa, r_qTa = sbt(e2, "qTa", [128, 8, TS], BF16)
                                qTbb, r_qTbb = sbt(e2, "qTbb", [128, 8, TS], BF16)
                                qTo, r_qTo = qTbb, r_qTbb
                                r_kmc = [Res() for _ in range(8)]
                                r_vmc = [Res() for _ in range(8)]
                                KTm = Rot([sbt(e2, f"KTm{i}", [128, NT], BF16) for i in range(1)])
                                Vm = Rot([sbt(e2, f"Vm{i}", [128, NT // 128, 128], BF16) for i in range(1)])
                                eb = Rot([sbt(e2, f"eb{i}", [128, TS], F32) for i in range(2)])
                                Lb = Rot([sbt(e2, f"Lb{i}", [128, TS], BF16) for i in range(4)])
                                ab = Rot([sbt(e2, f"ab{i}", [128, TS], BF16) for i in range(4)])
                                Lacc32s = [sbt(e2, f"Lacc32_{i}", [128, TS], F32) for i in range(2)]
                                Lacc16s = [Rot([sbt(e2, f"Lacc16_{k}_{i}", [128, TS], BF16) for i in range(2)]) for k in range(2)]
                                pZ = pY
                                pO = Rot([pst(e2, f"pO{i}", [128, 512], F32) for i in range(2)])

                                def sb_group(streams, nq):
                                    ns_ = len(streams)
                                    nu = len(streams[0][3])
                                    S_ = []
                                    for si, (hd, q_ap, r_q, units, out_cols) in enumerate(streams):
                                        la, r_la = Lacc32s[si]
                                        I(POOL, lambda h: h.memset(la[:, :nq], 0.0), writes=[r_la])
                                        S_.append(dict(hd=hd, q=q_ap, rq=r_q, u=units, oc=out_cols, po=pO.next(), la=(la, r_la),
                                                       l16=None, pz={}, L={}, a={}, l16m={}))

                                    def A(d, i):
                                        kT_ap, v_ap, nk, mask, rds = d["u"][i]
                                        pz, r_pz = pZ.next()
                                        I(PE, lambda h: h.matmul(pz[:nk, :nq], lhsT=kT_ap, rhs=d["q"], start=True, stop=False, skip_group_check=True),
                                          reads=rds + [d["rq"]], writes=[r_pz])
                                        d["pz"][i] = (pz, r_pz)

                                    def B(d, i, si):
                                        kT_ap, v_ap, nk, mask, rds = d["u"][i]
                                        pz, r_pz = d["pz"][i]
                                        e_, r_e = eb.next()
                                        L_, r_L = Lb.next()
                                        I(ACT, lambda h: h.activation(out=e_[:nk, :nq], in_=pz[:nk, :nq], func=AF.Exp), reads=[r_pz], writes=[r_e])
                                        I(ACT, lambda h: h.activation(out=L_[:nk, :nq], in_=e_[:nk, :nq], func=AF.Ln, bias=1.0, scale=1.0),
                                          reads=[r_e], writes=[r_L])
                                        if mask is not None:
                                            I(DVE, lambda h: h.tensor_tensor(out=L_[:nk, :nq], in0=L_[:nk, :nq], in1=mask[0], op=ALU.mult),
                                              reads=[r_L, mask[1]], writes=[r_L])
                                        d["L"][i] = (L_, r_L)
                                        if i < nu - 1:
                                            la, r_la = d["la"]
                                            I(DVE, lambda h: h.tensor_tensor(out=la[:nk, :nq], in0=la[:nk, :nq], in1=L_[:nk, :nq], op=ALU.add),
                                              reads=[r_la, r_L], writes=[r_la])
                                            l16, r_l16 = Lacc16s[si].next()
                                            ceng = DVE
                                            I(ceng, lambda h: h.tensor_copy(out=l16[:, :nq], in_=la[:, :nq]), reads=[r_la], writes=[r_l16])
                                            d["l16m"][i] = (l16, r_l16)

                                    def C(d, i):
                                        kT_ap, v_ap, nk, mask, rds = d["u"][i]
                                        pz, r_pz = d["pz"][i]
                                        L_, r_L = d["L"].pop(i)
                                        I(PE, lambda h: h.matmul(pz[:nk, :nq], lhsT=negtri[:nk, :nk], rhs=L_[:nk, :nq], start=False, stop=(i == 0),
                                                                 skip_group_check=True),
                                          reads=[r_negtri, r_L], writes=[r_pz])
                                        if i > 0:
                                            l16, r_l16 = d["l16m"].pop(i - 1)
                                            I(PE, lambda h: h.matmul(pz[:nk, :nq], lhsT=negones[:, :nk], rhs=l16[:, :nq], start=False, stop=True,
                                                                     skip_group_check=True),
                                              reads=[r_negones, r_l16], writes=[r_pz])

                                    def Dd(d, i):
                                        kT_ap, v_ap, nk, mask, rds = d["u"][i]
                                        pz, r_pz = d["pz"].pop(i)
                                        a_, r_a = ab.next()
                                        I(ACT, lambda h: h.activation(out=a_[:nk, :nq], in_=pz[:nk, :nq], func=AF.Exp), reads=[r_pz], writes=[r_a])
                                        if mask is not None:
                                            I(DVE, lambda h: h.tensor_tensor(out=a_[:nk, :nq], in0=a_[:nk, :nq], in1=mask[0], op=ALU.mult),
                                              reads=[r_a, mask[1]], writes=[r_a])
                                        d["a"][i] = (a_, r_a)

                                    def E(d, i):
                                        kT_ap, v_ap, nk, mask, rds = d["u"][i]
                                        a_, r_a = d["a"].pop(i)
                                        po, r_po = d["po"]
                                        I(PE, lambda h: h.matmul(po[:, :nq], lhsT=v_ap, rhs=a_[:nk, :nq], start=(i == 0), stop=(i == nu - 1)),
                                          reads=rds + [r_a], writes=[r_po])

                                    for d in S_:
                                        A(d, 0)
                                    for si, d in enumerate(S_):
                                        B(d, 0, si)
                                    def LAev(d, i, si):
                                        la, r_la = d["la"]
                                        l16, r_l16 = Lacc16s[si].next()
                                        I(DVE, lambda h: h.tensor_copy(out=l16[:, :nq], in_=la[:, :nq]), reads=[r_la], writes=[r_l16])
                                        d["l16m"][i] = (l16, r_l16)

                                    for i in range(nu):
                                        for si, d in enumerate(S_):
                                            C(d, i)
                                            if i + 1 < nu:
                                                A(d, i + 1)
                                            Dd(d, i)
                                            if i + 1 < nu:
                                                B(d, i + 1, si)
                                            E(d, i)
                                    for d in S_:
                                        m, r0 = d["hd"] // 2, (d["hd"] % 2) * 64
                                        po, r_po = d["po"]
                                        evac(OTs[r0:r0 + 64, m, d["oc"]], po[r0:r0 + 64, :nq], [r_po], [r_OTs])

                                for j in range(8):
                                    ta, tb = 2 * j, 2 * j + 1
                                    I(DSP, lambda h: h.dma_start(out=qTa[:], in_=QT[1][:, :, ta * TS:(ta + 1) * TS].rearrange("m p t -> p m t")),
                                      reads=[r_QT[1][ta]], writes=[r_qTa])
                                    I(DSP, lambda h: h.dma_start(out=qTbb[:], in_=QT[1][:, :, tb * TS:(tb + 1) * TS].rearrange("m p t -> p m t")),
                                      reads=[r_QT[1][tb]], writes=[r_qTbb])
                                    I(DVE, lambda h: h.tensor_scalar(out=qTa[:], in0=qTa[:], scalar1=pm[:, 0:1], scalar2=None, op0=ALU.mult),
                                      reads=[r_qTa, r_pm], writes=[r_qTa])
                                    I(DVE, lambda h: h.scalar_tensor_tensor(out=qTo[:], in0=qTbb[:], scalar=pm[:, 1:2], in1=qTa[:],
                                                                            op0=ALU.mult, op1=ALU.add),
                                      reads=[r_qTa, r_pm], writes=[r_qTo])
                                    nk_all = (2 * j + 2) * TS
                                    nkt = nk_all // 128
                                    for m in range(8):
                                        km, _ = KTm.next()
                                        vm, _ = Vm.next()
                                        nch = nk_all // 1024
                                        for c in range(nch - 1, -1, -1):
                                            k0, k1 = c * 1024, (c + 1) * 1024
                                            I(DSP, lambda h: h.dma_start(out=km[:, k0:k1], in_=KT[1][m, :, k0:k1]),
                                              reads=[r_KT[1][2 * c], r_KT[1][2 * c + 1]], writes=[r_kmc[c]])
                                            I(DSP, lambda h: h.dma_start(out=vm[:, k0 // 128:k1 // 128, :], in_=VBm[m, :, k0 // 128:k1 // 128, :]),
                                              reads=[r_VB[1][2 * c], r_VB[1][2 * c + 1]], writes=[r_vmc[c]])
                                        streams = []
                                        for hh in range(2):
                                            hd = 2 * m + hh
                                            r0 = hh * 64
                                            units = []
                                            for kt in range(nkt - 1, -1, -1):
                                                krel = kt - 8 * j
                                                mask = (msb[:, krel, :], r_msb) if krel >= 0 else None
                                                units.append((km[r0:r0 + 64, kt * 128:(kt + 1) * 128], vm[:, kt, :], 128, mask,
                                                              [r_kmc[kt // 8], r_vmc[kt // 8]]))
                                            streams.append((hd, qTo[r0:r0 + 64, m, :], r_qTo, units, slice(0, TS)))
                                        sb_group(streams, TS)

                                    def xload(sub, j=j):
                                        xa, r_xa = xbuf.next()
                                        xb_, r_xb_ = xbuf.next()
                                        ra = (2 * j) * TS + sub * 128
                                        rb = (2 * j + 1) * TS + sub * 128
                                        I(DSP, lambda h: h.dma_start(out=xa[:, :], in_=X2[ra:ra + 128, :]), reads=[r_X2[2 * j]], writes=[r_xa])
                                        I(DSP, lambda h: h.dma_start(out=xb_[:, :], in_=X2[rb:rb + 128, :]), reads=[r_X2[2 * j + 1]], writes=[r_xb_])
                                        I(DVE, lambda h: h.tensor_scalar(out=xa[:, :], in0=xa[:, :], scalar1=pm[:, 0:1], scalar2=None, op0=ALU.mult),
                                          reads=[r_xa, r_pm], writes=[r_xa])
                                        I(DVE, lambda h: h.scalar_tensor_tensor(out=xb_[:, :], in0=xb_[:, :], scalar=pm[:, 1:2], in1=xa[:, :],
                                                                                op0=ALU.mult, op1=ALU.add),
                                          reads=[r_xa, r_xb_, r_pm], writes=[r_xb_])
                                        return xb_, r_xb_
                                    post_attn(j, 4, 128, xload, 0, j * 4)

                                ckb, r_ckb = sbt(e2, "ckb1", [128, D], BF16)
                                ckT, r_ckT = sbt(e2, "ckT1", [128, 8, 128], BF16)
                                pT2 = nb[4]
                                for s in range(2):
                                    for kt in range(32):
                                        I(DPOOL, lambda h: h.dma_start(out=ckb[:], in_=cbk[s, kt * 128:(kt + 1) * 128, :]), writes=[r_ckb])
                                        pt, r_pt = pT2.next()
                                        for m in range(8):
                                            I(PE, lambda h: h.transpose(out=pt[:, m, :], in_=ckb[:, m * 128:(m + 1) * 128], identity=identb[:]),
                                              reads=[r_ckb, r_identb], writes=[r_pt])
                                        evac(ckT[:], pt[:], [r_pt], [r_ckT])
                                        I(DSP, lambda h: h.dma_start(out=CKT[s, :, :, kt * 128:(kt + 1) * 128].rearrange("m p t -> p m t"), in_=ckT[:]),
                                          reads=[r_ckT], writes=[r_CKT[s]])
                                nKT, r_nKT = sbt(e2, "nKT1", [128, 8, 32], BF16)
                                nV, r_nV = sbt(e2, "nV1", [32, D], BF16)
                                qTs, r_qTs = sbt(e2, "qTs1", [128, 8, 32], BF16)
                                for s in range(2):
                                    c0 = NT + s * 32
                                    I(DSP, lambda h: h.dma_start(out=nKT[:], in_=KT[1][:, :, c0:c0 + 32].rearrange("m p t -> p m t")),
                                      reads=[r_KT[1][16]], writes=[r_nKT])
                                    I(DSP, lambda h: h.dma_start(out=nV[:, :].rearrange("p (m f) -> p m f", f=128),
                                                                 in_=VBm[:, s * 32:(s + 1) * 32, NT // 128, :].rearrange("m p f -> p m f")),
                                      reads=[r_VB[1][16]], writes=[r_nV])
                                    I(DSP, lambda h: h.dma_start(out=qTs[:], in_=QT[1][:, :, c0:c0 + 32].rearrange("m p t -> p m t")),
                                      reads=[r_QT[1][16]], writes=[r_qTs])
                                    for m in range(8):
                                        km, _ = KTm.next()
                                        vm, _ = Vm.next()
                                        for c in range(3, -1, -1):
                                            k0, k1 = c * 1024, (c + 1) * 1024
                                            I(DSP, lambda h: h.dma_start(out=km[:, k0:k1], in_=CKT[s, m, :, k0:k1]), reads=[r_CKT[s]], writes=[r_kmc[c]])
                                            I(DPOOL, lambda h: h.dma_start(
                                                out=vm[:, k0 // 128:k1 // 128, :],
                                                in_=cbv[s, k0:k1, m * 128:(m + 1) * 128].rearrange("(k p) f -> p k f", p=128)),
                                              writes=[r_vmc[c]])
                                        streams = []
                                        for hh in range(2):
                                            hd = 2 * m + hh
                                            r0 = hh * 64
                                            units = [(nKT[r0:r0 + 64, m, :], nV[:, m * 128:(m + 1) * 128], 32, (msmp[:, :], r_msmp), [r_nKT, r_nV])]
                                            for kt in range(31, -1, -1):
                                                units.append((km[r0:r0 + 64, kt * 128:(kt + 1) * 128], vm[:, kt, :], 128, None,
                                                              [r_kmc[kt // 8], r_vmc[kt // 8]]))
                                            streams.append((hd, qTs[r0:r0 + 64, m, :], r_qTs, units, slice(s * 32, (s + 1) * 32)))
                                        sb_group(streams, 32)

                                def xload_s(sub):
                                    xb, r_xb = xbuf.next()
                                    I(DSP, lambda h: h.dma_start(out=xb[:SMP, :], in_=X2[NT:NT + SMP, :]), reads=[r_X2[16]], writes=[r_xb])
                                    return xb, r_xb
                                post_attn(8, 1, SMP, xload_s, 1, 32)
                                S.barrier()
                    if stop_after == ("A2", l):
                        break

                with ExitStack() as es:
                    n_exp = 1 if l == 0 else NE
                    wgs, wus, wds = (wgd, wud, wdd) if l == 0 else (wge, wue, wde)
                    ngroups = 4 if l == 0 else 2
                    x_res, r_xres = (X1, r_X1) if l == 0 else (X3, r_X3)
                    samp_row = NT if l == 0 else NT // 2
                    xg, r_xg = sbt(es, "xg", [128, 8, 2048 + SMP], BF16)
                    acc, r_acc = sbt(es, "acc", [128, 17, D], F32)
                    wgb = Rot([sbt(es, f"wgb{i}", [128, 8, 512], BF16) for i in range(2)])
                    wub = Rot([sbt(es, f"wub{i}", [128, 8, 512], BF16) for i in range(2)])
                    wdb = Rot([sbt(es, f"wdb{i}", [128, 4, D], BF16) for i in range(2)])
                    hTb = Rot([sbt(es, f"hTb{i}", [128, 4, TS], BF16) for i in range(2)])
                    sgb = Rot([sbt(es, f"sgb{i}", [128, TS], F32) for i in range(2)])
                    xfb = Rot([sbt(es, f"xfb{i}", [128, D], F32) for i in range(2)])
                    pG = Rot([pst(es, f"pG{i}", [128, 512], F32) for i in range(2)])
                    pU = Rot([pst(es, f"pU{i}", [128, 512], F32) for i in range(2)])
                    pYf = Rot([pst(es, f"pYf{i}", [128, 512], F32) for i in range(3)])
                    for g in range(ngroups):
                        last = (g == ngroups - 1)
                        tiles = [(g * 4 + i, i * TS, TS, 4, 128) for i in range(4)]
                        if last:
                            tiles.append((16 if l == 0 else 8, 2048, SMP, 1, SMP))
                        ncol = 2048 + (SMP if last else 0)
                        rds = [r_XN2T[l][tid] for (tid, _, _, _, _) in tiles]
                        I(DSP, lambda h: h.dma_start(out=xg[:, :, 0:2048],
                                                     in_=XN2T[l][:, :, g * 2048:(g + 1) * 2048].rearrange("m p t -> p m t")),
                          reads=rds[:4], writes=[r_xg])
                        if last:
                            I(DSP, lambda h: h.dma_start(out=xg[:, :, 2048:2048 + SMP],
                                                         in_=XN2T[l][:, :, samp_row:samp_row + SMP].rearrange("m p t -> p m t")),
                              reads=rds[4:], writes=[r_xg])
                        first_acc = True
                        for e in range(n_exp):
                            for fc in range(NFC):
                                wg_, r_wg = wgb.next()
                                wu_, r_wu = wub.next()
                                wd_, r_wd = wdb.next()
                                fsl = slice(fc * 512, (fc + 1) * 512)
                                I(DPOOL, lambda h: h.dma_start(out=wg_[:], in_=wgs[e, :, fsl].rearrange("(k p) f -> p k f", p=128)), writes=[r_wg])
                                I(DPOOL, lambda h: h.dma_start(out=wu_[:], in_=wus[e, :, fsl].rearrange("(k p) f -> p k f", p=128)), writes=[r_wu])
                                I(DPOOL, lambda h: h.dma_start(out=wd_[:], in_=wds[e, fsl, :].rearrange("(s p) d -> p s d", p=128)), writes=[r_wd])
                                for ti, (tid, c0, ntok, nsub, P_) in enumerate(tiles):
                                    hT, r_hT = hTb.next()
                                    for fs in range(4):
                                        pg, r_pg = pG.next()
                                        pu, r_pu = pU.next()
                                        for k in range(8):
                                            I(PE, lambda h: h.matmul(pg[:, :ntok], lhsT=wg_[:, k, fs * 128:(fs + 1) * 128], rhs=xg[:, k, c0:c0 + ntok],
                                                                     start=(k == 0), stop=(k == 7)), reads=[r_wg, r_xg], writes=[r_pg])
                                        for k in range(8):
                                            I(PE, lambda h: h.matmul(pu[:, :ntok], lhsT=wu_[:, k, fs * 128:(fs + 1) * 128], rhs=xg[:, k, c0:c0 + ntok],
                                                                     start=(k == 0), stop=(k == 7)), reads=[r_wu, r_xg], writes=[r_pu])
                                        sg_, r_sg = sgb.next()
                                        I(ACT, lambda h: h.activation(out=sg_[:, :ntok], in_=pg[:, :ntok], func=AF.Silu), reads=[r_pg], writes=[r_sg])
                                        I(DVE, lambda h: h.tensor_tensor(out=hT[:, fs, :ntok], in0=pu[:, :ntok], in1=sg_[:, :ntok], op=ALU.mult),
                                          reads=[r_pu, r_sg], writes=[r_hT])
                                    for sub in range(nsub):
                                        sa = ti * 4 + sub
                                        sgl = (g * 16 + sa) if tid < (16 if l == 0 else 8) else 32
                                        for half in range(2):
                                            py, r_py = pYf.next()
                                            for fs in range(4):
                                                I(PE, lambda h: h.matmul(py[:P_, :], lhsT=hT[:, fs, sub * 128: sub * 128 + P_],
                                                                         rhs=wd_[:, fs, half * 512:(half + 1) * 512], start=(fs == 0), stop=(fs == 3)),
                                                  reads=[r_hT, r_wd], writes=[r_py])
                                            dst = acc[:P_, sa, half * 512:(half + 1) * 512]
                                            gsc = gates[:P_, sgl, e:e + 1] if l == 1 else 1.0
                                            grd = [r_gates] if l == 1 else []
                                            if first_acc:
                                                I(DVE, lambda h: h.tensor_scalar(out=dst, in0=py[:P_, :], scalar1=gsc, scalar2=None, op0=ALU.mult),
                                                  reads=[r_py] + grd, writes=[r_acc])
                                            else:
                                                I(DVE, lambda h: h.scalar_tensor_tensor(out=dst, in0=py[:P_, :], scalar=gsc, in1=dst,
                                                                                        op0=ALU.mult, op1=ALU.add),
                                                  reads=[r_py, r_acc] + grd, writes=[r_acc])
                                first_acc = False
                        for ti, (tid, c0, ntok, nsub, P_) in enumerate(tiles):
                            set_i = 1 if P_ == SMP else 0
                            for sub in range(nsub):
                                sa = ti * 4 + sub
                                rr = (tid * TS + sub * 128) if P_ == 128 else samp_row
                                xf, r_xf = xfb.next()
                                I(DSP, lambda h: h.dma_start(out=xf[:P_, :], in_=x_res[rr:rr + P_, :]), reads=[r_xres[tid]], writes=[r_xf])
                                I(POOL, lambda h: h.tensor_tensor(out=acc[:P_, sa, :], in0=acc[:P_, sa, :], in1=G2[set_i][0][:P_, :], op=ALU.mult),
                                  reads=[r_acc, G2[set_i][1]], writes=[r_acc])
                                I(POOL, lambda h: h.tensor_tensor(out=xf[:P_, :], in0=xf[:P_, :], in1=acc[:P_, sa, :], op=ALU.add),
                                  reads=[r_acc, r_xf], writes=[r_xf])
                                if l == 0:
                                    I(DSP, lambda h: h.dma_start(out=X2[rr:rr + P_, :], in_=xf[:P_, :]), reads=[r_xf], writes=[r_X2[tid]])
                                else:
                                    if P_ == 128:
                                        I(DSP, lambda h: h.dma_start(out=yp[rr:rr + P_, :], in_=xf[:P_, :]), reads=[r_xf], writes=[r_out])
                                    else:
                                        I(DSP, lambda h: h.dma_start(out=ys[:, :], in_=xf[:P_, :]), reads=[r_xf], writes=[r_out])
                    S.barrier()

        S.final_wait(S.q_sp, [S.DSP, S.DPOOL, S.PE, S.ACT, S.DVE, S.POOL])
        print(f"[build] instructions={S.ninst} waits={S.nwaits}", flush=True)
    return nc


_NC_CACHE = {}
_DEBUG_HOOK = []


def _host_consts():
    ident = np.eye(128, dtype=np.float32)
    j = np.arange(128)[:, None]
    s = np.arange(128)[None, :]
    negtri = np.where(j >= s, -1.0, 0.0).astype(np.float32)
    msmp = (np.arange(32)[:, None] < np.arange(32)[None, :]).astype(np.float32)
    return ident, negtri, msmp


def _msb_for(p):
    k = np.arange(128)[:, None, None]
    kr = np.arange(8)[None, :, None]
    q = np.arange(512)[None, None, :]
    return ((kr * 128 + k) < (p * 512 + q)).astype(np.float32)


def kernel(x_prompt, x_sample, cache_a_k, cache_a_v, cache_b_k, cache_b_v, c_prompt, c_sample,
           w_qkv, w_o, norm1_g, norm2_g, w_ada, b_ada, q_norm_g, k_norm_g, rel_bias,
           w_gate_d, w_up_d, w_down_d, w_router, w_gate_e, w_up_e, w_down_e):
    f32 = lambda a: np.ascontiguousarray(np.asarray(a, dtype=np.float32))
    x_prompt, x_sample = f32(x_prompt), f32(x_sample)
    cache_a_k, cache_a_v, cache_b_k, cache_b_v = f32(cache_a_k), f32(cache_a_v), f32(cache_b_k), f32(cache_b_v)
    c_prompt, c_sample = f32(c_prompt), f32(c_sample)
    rel_bias = f32(rel_bias)
    kk = np.arange(128)[:, None]
    jj = np.arange(WRELW)[None, :]
    idx = np.clip(jj - 127 - kk, -128, 128) + 128
    wrel = np.ascontiguousarray(rel_bias[0][:, idx])
    ident, negtri, msmp = _host_consts()
    shared = dict(
        w_qkv=f32(w_qkv), w_o=f32(w_o), n1g=f32(norm1_g), n2g=f32(norm2_g), w_ada=f32(w_ada), b_ada=f32(b_ada),
        qng=f32(q_norm_g), kng=f32(k_norm_g), wrel=wrel, wgd=f32(w_gate_d), wud=f32(w_up_d), wdd=f32(w_down_d),
        wrt=f32(w_router)[0], wge=f32(w_gate_e)[0], wue=f32(w_up_e)[0], wde=f32(w_down_e)[0],
        ident=ident, negtri=negtri, msmp=msmp)
    in_maps = []
    for c in range(8):
        b, p = c // 2, c % 2
        m = dict(shared)
        m["xp"] = x_prompt[b]
        m["xs"] = np.ascontiguousarray(x_sample[2 * c:2 * c + 2].reshape(SMP, D))
        m["cak"] = np.ascontiguousarray(cache_a_k[0, 2 * c:2 * c + 2].reshape(2, 512, D))
        m["cav"] = np.ascontiguousarray(cache_a_v[0, 2 * c:2 * c + 2].reshape(2, 512, D))
        m["cbk"] = np.ascontiguousarray(cache_b_k[0, 2 * c:2 * c + 2].reshape(2, 4096, D))
        m["cbv"] = np.ascontiguousarray(cache_b_v[0, 2 * c:2 * c + 2].reshape(2, 4096, D))
        m["cpl"] = np.ascontiguousarray(c_prompt[b].reshape(8, 128).T)
        m["csl"] = np.ascontiguousarray(c_sample[2 * c:2 * c + 2].reshape(2, 8, 128).transpose(2, 1, 0))
        m["msb"] = _msb_for(p)
        pmv = np.zeros((128, 2), np.float32)
        pmv[:, 0] = 1.0 - p
        pmv[:, 1] = float(p)
        m["pm"] = pmv
        in_maps.append(m)
    if _DEBUG_HOOK:
        return _DEBUG_HOOK[0](in_maps)
    if "nc" not in _NC_CACHE:
        _NC_CACHE["nc"] = build_program()
    res = run_bass_kernel_spmd(_NC_CACHE["nc"], in_maps, core_ids=list(range(8)))
    R = res.results
    B, SQ = 4, NT
    y_prompt = np.zeros((B, SQ, D), np.float32)
    y_sample = np.zeros((16, 32, D), np.float32)
    a_k_p = np.zeros((1, B, 512, 16, 64), np.float32)
    a_v_p = np.zeros_like(a_k_p)
    a_k_s = np.zeros((1, 16, 512, 16, 64), np.float32)
    a_v_s = np.zeros_like(a_k_s)
    b_k_p = np.zeros((1, B, SQ, 16, 64), np.float32)
    b_v_p = np.zeros_like(b_k_p)
    b_k_s = np.zeros((1, 16, 32, 16, 64), np.float32)
    b_v_s = np.zeros_like(b_k_s)
    for c in range(8):
        b, p = c // 2, c % 2
        r = R[c]
        ypc = np.asarray(r["yp"]).reshape(8, TS, D)
        for j in range(8):
            t = 2 * j + p
            y_prompt[b, t * TS:(t + 1) * TS] = ypc[j]
        y_sample[2 * c:2 * c + 2] = np.asarray(r["ys"]).reshape(2, 32, D)
        a_k_s[0, 2 * c:2 * c + 2] = np.asarray(r["aks"]).reshape(2, 512, 16, 64)
        a_v_s[0, 2 * c:2 * c + 2] = np.asarray(r["avs"]).reshape(2, 512, 16, 64)
        b_k_s[0, 2 * c:2 * c + 2] = np.asarray(r["bks"]).reshape(2, 32, 16, 64)
        b_v_s[0, 2 * c:2 * c + 2] = np.asarray(r["bvs"]).reshape(2, 32, 16, 64)
        if p == 0:
            a_k_p[0, b] = np.asarray(r["akp"]).reshape(512, 16, 64)
            a_v_p[0, b] = np.asarray(r["avp"]).reshape(512, 16, 64)
        half = slice(p * (SQ // 2), (p + 1) * (SQ // 2))
        b_k_p[0, b, half] = np.asarray(r["bkp"]).reshape(SQ, 16, 64)[half]
        b_v_p[0, b, half] = np.asarray(r["bvp"]).reshape(SQ, 16, 64)[half]
    return (y_prompt, y_sample, a_k_p, a_v_p, a_k_s, a_v_s, b_k_p, b_v_p, b_k_s, b_v_s)
```
